# Optimizing a Trainium2 kernel written in Bass

```python
import math
import jax, jax.numpy as jnp
from jax import lax
import numpy as np

D_MODEL = 1024
BATCH = 8
SEQ = 2048
DEPTH = 2

D_PLE = 256
D_POOL = 512
D_CONV = D_MODEL - D_POOL
POOL_WINDOWS = (2, 4, 8, 16)
N_POOL_GROUPS = len(POOL_WINDOWS)
POOL_GROUP_DIM = D_POOL // N_POOL_GROUPS
CONV_WIDTH = 31
D_IN_PROJ = D_POOL + 2 * D_CONV
D_FF_DENSE = 2816
N_EXPERTS = 8
TOP_K = 2
D_FF_EXPERT = 3584
N_DENSE = (DEPTH + 1) // 2
N_MOE = DEPTH // 2
DEEPNORM_ALPHA = (2.0 * DEPTH) ** 0.25
DEEPNORM_BETA = (8.0 * DEPTH) ** -0.25
LN_EPS = 1e-5

kernel_name = "hybrid_pool_conformer_moe_deepnorm"


def layer_norm(x, g, b):
    xf = x.astype(jnp.float32)
    mu = jnp.mean(xf, axis=-1, keepdims=True)
    var = jnp.mean(jnp.square(xf - mu), axis=-1, keepdims=True)
    y = (xf - mu) * lax.rsqrt(var + LN_EPS)
    return (y * g.astype(jnp.float32) + b.astype(jnp.float32)).astype(x.dtype)


def causal_multiscale_pool(u):
    B, S, _ = u.shape
    uf = u.astype(jnp.float32).reshape(B, S, N_POOL_GROUPS, POOL_GROUP_DIM)
    cs = jnp.cumsum(uf, axis=1)
    pos = jnp.arange(S, dtype=jnp.float32)[None, :, None]
    outs = []
    for gi, w in enumerate(POOL_WINDOWS):
        c = cs[:, :, gi]
        prev = jnp.pad(c, ((0, 0), (w, 0), (0, 0)))[:, :S]
        count = jnp.minimum(pos + 1.0, float(w))
        outs.append((c - prev) / count - uf[:, :, gi])
    return jnp.stack(outs, axis=2).astype(u.dtype)


def pool_mixer(u, w_grp, scale):
    d = causal_multiscale_pool(u)
    y = jnp.einsum('bsgc,gcd->bsgd', d, w_grp)
    return y.reshape(u.shape) * scale


def conformer_conv(a, gate, w_dw, b_dw, g_ln, b_ln, w_pw):
    v = a * jax.nn.sigmoid(gate)
    vp = jnp.pad(v, ((0, 0), (CONV_WIDTH - 1, 0), (0, 0)))
    y = lax.conv_general_dilated(
        vp, w_dw[:, None, :].astype(v.dtype), window_strides=(1,), padding='VALID',
        dimension_numbers=('NWC', 'WIO', 'NWC'), feature_group_count=D_CONV) + b_dw
    y = jax.nn.silu(layer_norm(y, g_ln, b_ln))
    return y @ w_pw


def swiglu(h, w1, w3, w2):
    return (jax.nn.silu(h @ w1) * (h @ w3)) @ w2


def moe_swiglu(h, w_router, w1, w3, w2):
    B, S, D = h.shape
    t = h.reshape(B * S, D)
    logits = (t @ w_router).astype(jnp.float32)
    top_vals, top_idx = lax.top_k(logits, TOP_K)
    top_w = jax.nn.softmax(top_vals, axis=-1)
    gates = jnp.sum(jax.nn.one_hot(top_idx, N_EXPERTS, dtype=jnp.float32) * top_w[..., None],
                    axis=1).astype(h.dtype)
    out = jnp.zeros_like(t)
    for e in range(N_EXPERTS):
        out = out + gates[:, e:e + 1] * swiglu(t, w1[e], w3[e], w2[e])
    return out.reshape(B, S, D)


def setup_inputs(seed: int = 0) -> dict:
    key = jax.random.key(seed)
    ks = iter(jax.random.split(key, 32))
    f32 = jnp.float32

    def nrm(shape, scale):
        return jax.random.normal(next(ks), shape, f32) * scale

    def gain(shape):
        return 1.0 + nrm(shape, 0.02)

    L = DEPTH
    return {
        "x": nrm((BATCH, SEQ, D_MODEL), 1.0),
        "p": nrm((DEPTH, BATCH, SEQ, D_PLE), 1.0),
        "w_in": nrm((L, D_MODEL, D_IN_PROJ), D_MODEL ** -0.5),
        "pool_w": nrm((L, N_POOL_GROUPS, POOL_GROUP_DIM, POOL_GROUP_DIM), POOL_GROUP_DIM ** -0.5),
        "pool_scale": gain((L, D_POOL)),
        "conv_w": nrm((L, CONV_WIDTH, D_CONV), CONV_WIDTH ** -0.5),
        "conv_b": nrm((L, D_CONV), 0.02),
        "conv_ln_g": gain((L, D_CONV)),
        "conv_ln_b": nrm((L, D_CONV), 0.02),
        "conv_pw": nrm((L, D_CONV, D_CONV), D_CONV ** -0.5),
        "w_out": nrm((L, D_MODEL, D_MODEL), DEEPNORM_BETA * D_MODEL ** -0.5),
        "ln1_g": gain((L, D_MODEL)),
        "ln1_b": nrm((L, D_MODEL), 0.02),
        "dense_w1": nrm((N_DENSE, D_MODEL, D_FF_DENSE), D_MODEL ** -0.5),
        "dense_w3": nrm((N_DENSE, D_MODEL, D_FF_DENSE), D_MODEL ** -0.5),
        "dense_w2": nrm((N_DENSE, D_FF_DENSE, D_MODEL), DEEPNORM_BETA * D_FF_DENSE ** -0.5),
        "router_w": nrm((N_MOE, D_MODEL, N_EXPERTS), D_MODEL ** -0.5),
        "exp_w1": nrm((N_MOE, N_EXPERTS, D_MODEL, D_FF_EXPERT), D_MODEL ** -0.5),
        "exp_w3": nrm((N_MOE, N_EXPERTS, D_MODEL, D_FF_EXPERT), D_MODEL ** -0.5),
        "exp_w2": nrm((N_MOE, N_EXPERTS, D_FF_EXPERT, D_MODEL), DEEPNORM_BETA * D_FF_EXPERT ** -0.5),
        "ln2_g": gain((L, D_MODEL)),
        "ln2_b": nrm((L, D_MODEL), 0.02),
        "ple_gate_w": nrm((L, D_MODEL, D_MODEL), D_MODEL ** -0.5),
        "ple_w": nrm((L, D_PLE, D_MODEL), DEEPNORM_BETA * D_PLE ** -0.5),
    }


def reference(x, p, w_in, pool_w, pool_scale, conv_w, conv_b, conv_ln_g, conv_ln_b, conv_pw,
              w_out, ln1_g, ln1_b, dense_w1, dense_w3, dense_w2, router_w, exp_w1, exp_w3,
              exp_w2, ln2_g, ln2_b, ple_gate_w, ple_w):
    for i in range(DEPTH):
        u = x @ w_in[i]
        u_pool = u[..., :D_POOL]
        u_val = u[..., D_POOL:D_POOL + D_CONV]
        u_gate = u[..., D_POOL + D_CONV:]
        y_pool = pool_mixer(u_pool, pool_w[i], pool_scale[i])
        y_conv = conformer_conv(u_val, u_gate, conv_w[i], conv_b[i],
                                conv_ln_g[i], conv_ln_b[i], conv_pw[i])
        mix = jnp.concatenate([y_pool, y_conv], axis=-1) @ w_out[i]
        x = layer_norm(DEEPNORM_ALPHA * x + mix, ln1_g[i], ln1_b[i])
        j = i // 2
        if i % 2 == 0:
            f = swiglu(x, dense_w1[j], dense_w3[j], dense_w2[j])
        else:
            f = moe_swiglu(x, router_w[j], exp_w1[j], exp_w3[j], exp_w2[j])
        x = layer_norm(DEEPNORM_ALPHA * x + f, ln2_g[i], ln2_b[i])
        x = x + jax.nn.sigmoid(x @ ple_gate_w[i]) * (p[i] @ ple_w[i])
    return x
```

```python
import numpy as np
from contextlib import ExitStack
import concourse.bass as bass
import concourse.mybir as mybir
from concourse.bass_utils import run_bass_kernel_spmd

F32 = mybir.dt.float32
BF16 = mybir.dt.bfloat16
I32 = mybir.dt.int32
AF = mybir.ActivationFunctionType
ALU = mybir.AluOpType

D = 1024
KC = D // 128
DPOOL = 512
DCONV = 512
NGRP = 4
WINS = (2, 4, 8, 16)
CW = 31
DIN = 1536
DPLE = 256
LN_EPS = 1e-5
HALO_P = 16
HALO_C = 32
NPP = 16 + 4 * CW


def const_offsets(NSB, NG13, NJG):
    o = {}
    n = 256 + NGRP * HALO_P
    for k, w in (("tri", 128), ("biota", NSB), ("c13", NG13), ("c2", 2 * NJG)):
        o[k] = n
        n += w
    o["n"] = n
    return o


class Cfg:
    def __init__(self, S=2048, FFD=2816, E=8, FFE=3584, L=2):
        self.S, self.FFD, self.E, self.FFE, self.L = S, FFD, E, FFE, L
        self.NT = S // 128
        self.NB = S // 512
        self.alpha = (2.0 * L) ** 0.25


class Res:
    __slots__ = ("n", "lw", "rd", "x")

    def __init__(self, n, x=False):
        self.n, self.lw, self.rd, self.x = n, None, {}, x


class Eng:
    def __init__(self, name, h, si):
        self.name, self.h, self.si, self.seen = name, h, si, {}


class Prog:
    def __init__(self):
        self.nc = bass.Bass("TRN2", target_bir_lowering=False)
        self.st = ExitStack()
        self.sems, self.semval, self.allres = [], [], []
        nc = self.nc
        self.eng = {}
        for name, h in (("pe", nc.tensor), ("act", nc.scalar), ("dve", nc.vector),
                        ("pool", nc.gpsimd), ("sp", nc.sync)):
            self.eng[name] = Eng(name, h, self.newsem("e_" + name))
        self.nwaits = 0
        self.sem_owner = {}

    def newsem(self, name):
        h = self.st.enter_context(self.nc.semaphore(name))
        self.sems.append(h)
        self.semval.append(0)
        return len(self.sems) - 1

    def res(self, name, x=False):
        r = Res(name, x)
        self.allres.append(r)
        return r

    def _deps(self, r, w):
        deps = {}

        def need(ev):
            if ev is not None and deps.get(ev[0], 0) < ev[1]:
                deps[ev[0]] = ev[1]
        for x in r:
            need(x.lw)
            if x.x:
                for ev in x.rd.items():
                    need(ev)
        for x in w:
            need(x.lw)
            for ev in x.rd.items():
                need(ev)
        return deps

    def _wait(self, eng, deps):
        for si, val in deps.items():
            if eng.name == "pe" and si == eng.si:
                continue
            if eng.seen.get(si, 0) < val:
                eng.h.wait_ge(self.sems[si], val)
                eng.seen[si] = val
                self.nwaits += 1

    def _record(self, ev, r, w):
        for x in w:
            x.lw, x.rd = ev, {}
        for x in r:
            if x.x:
                x.lw, x.rd = ev, {}
            elif x.rd.get(ev[0], 0) < ev[1]:
                x.rd[ev[0]] = ev[1]

    def op(self, e, fn, r=(), w=()):
        eng = self.eng[e]
        self._wait(eng, self._deps(r, w))
        inst = fn(eng.h)
        inst.then_inc(self.sems[eng.si], 1)
        self.semval[eng.si] += 1
        self._record((eng.si, self.semval[eng.si]), r, w)

    def dma(self, q, out, in_, sem, r=(), w=(), **kw):
        eng = self.eng[q]
        self._wait(eng, self._deps(r, w))
        eng.h.dma_start(out=out, in_=in_, **kw).then_inc(self.sems[sem], 16)
        self.sem_owner[sem] = q
        self.semval[sem] += 16
        self._record((sem, self.semval[sem]), r, w)

    def idma(self, out, in_, sem, out_off=None, in_off=None, r=(), w=()):
        eng = self.eng["pool"]
        self._wait(eng, self._deps(r, w))
        oo = bass.IndirectOffsetOnAxis(ap=out_off, axis=0) if out_off is not None else None
        io = bass.IndirectOffsetOnAxis(ap=in_off, axis=0) if in_off is not None else None
        eng.h.indirect_dma_start(out=out, out_offset=oo, in_=in_, in_offset=io).then_inc(self.sems[sem], 16)
        self.sem_owner[sem] = "pool"
        self.semval[sem] += 16
        self._record((sem, self.semval[sem]), r, w)

    def retire(self, sems):
        sems = set(sems)
        for x in self.allres:
            if x.lw is not None and x.lw[0] in sems:
                x.lw = None
            for si in [k for k in x.rd if k in sems]:
                del x.rd[si]
        for e in self.eng.values():
            for si in [k for k in e.seen if k in sems]:
                del e.seen[si]

    def cond(self, cond_expr, body, junk_out, junk_in, fix=None, retired=frozenset()):
        pre = list(self.semval)
        seen0 = {n: dict(e.seen) for n, e in self.eng.items()}
        with self.nc.If(cond_expr):
            body()
        post = list(self.semval)
        with self.nc.Else():
            for name, eng in self.eng.items():
                for si, owner in self.sem_owner.items():
                    if owner == name and post[si] != pre[si] and si not in retired:
                        eng.h.wait_ge(self.sems[si], pre[si])
                        if fix is not None and fix(eng, si, post[si] - pre[si]):
                            continue
                        eng.h.dma_start(out=junk_out[:, si:si + 1], in_=junk_in).then_inc(self.sems[si], post[si] - pre[si])
                d = post[eng.si] - pre[eng.si]
                if d:
                    eng.h.drain().then_inc(self.sems[eng.si], d)
        for n, e in self.eng.items():
            e.seen = seen0[n]

    def barrier(self):
        deps = {}
        for x in self.allres:
            if x.lw is not None and deps.get(x.lw[0], 0) < x.lw[1]:
                deps[x.lw[0]] = x.lw[1]
            for si, v in x.rd.items():
                if deps.get(si, 0) < v:
                    deps[si] = v
        for eng in self.eng.values():
            for si, val in deps.items():
                if eng.seen.get(si, 0) < val:
                    eng.h.wait_ge(self.sems[si], val)
                    eng.seen[si] = val
        self.allres = [x for x in self.allres if getattr(x, "keep", False) or True]


class Alias:
    def __init__(self, t, r):
        self.t, self.r = t, list(r)

    @property
    def r0(self):
        return self.r[0]


class Buf:
    _n = [0]

    def __init__(self, P, stack, name, shape, dt, nres=1):
        Buf._n[0] += 1
        name = f"{name}_{Buf._n[0]}"
        self.t = stack.enter_context(P.nc.sbuf_tensor(name, shape, dt))
        self.r = [P.res(f"{name}.{i}") for i in range(nres)]

    @property
    def r0(self):
        return self.r[0]


def build(cfg):
    P = Prog()
    nc = P.nc
    S, NT, NB, L, E = cfg.S, cfg.NT, cfg.NB, cfg.L, cfg.E
    alpha = cfg.alpha
    FFE_ = cfg.FFE

    def din(name, shape, dt=F32):
        return nc.dram_tensor(name, list(shape), dt, kind="ExternalInput").ap()

    x_d = din("x", [S, D])
    pT_d = din("pT", [L, DPLE, S])
    w_in_d = din("w_in", [L, D, DIN])
    pool_w_d = din("pool_w", [L, NGRP, 128, 128])
    conv_pw_d = din("conv_pw", [L, DCONV, DCONV])
    w_out_d = din("w_out", [L, D, D])
    ple_gate_d = din("ple_gate_w", [L, D, D])
    ple_w_d = din("ple_w", [L, DPLE, D])
    pp_d = din("pp", [128, L * NPP])
    lnb_d = din("lnb", [L * 4, D])
    NSB = (2 * S + E * 511) // 512
    NG13, NJG = FFE_ // 256, FFE_ // 512
    CO = const_offsets(NSB, NG13, NJG)
    consts_d = din("consts", [128, CO["n"]])
    tokid_d = din("tokid", [128, NT], I32)
    junk_d = din("junk", [128, 1])
    ND = (L + 1) // 2
    NM = L // 2
    FFD, FFE = cfg.FFD, cfg.FFE
    dw1_d = din("dense_w1", [ND, FFD // 256, 128, KC * 256])
    dw3_d = din("dense_w3", [ND, FFD // 256, 128, KC * 256])
    dw2_d = din("dense_w2", [ND, FFD, D])
    if NM:
        router_d = din("router_w", [NM, D, E])
        ew1_d = din("exp_w1", [NM * E * (FFE // 256) * 128, KC * 256])
        ew3_d = din("exp_w3", [NM * E * (FFE // 256) * 128, KC * 256])
        ew2_d = din("exp_w2", [NM * E * 2 * (FFE // 512) * 128, 4 * 512])
        X1D = nc.dram_tensor("x1d_scratch", [S, D], F32, kind="Internal").ap()
        SLOTMAP = nc.dram_tensor("slotmap_scratch", [NSB * 512, 1], I32, kind="Internal").ap()
        YD = nc.dram_tensor("yd_scratch", [NSB * 512, D], F32, kind="Internal").ap()
    out_d = nc.dram_tensor("out", [S, D], F32, kind="ExternalOutput").ap()

    st = P.st
    X = st.enter_context(nc.sbuf_tensor("X", [128, NT, D], F32))
    Xr = [P.res(f"X{t}") for t in range(NT)]
    CONST = Buf(P, st, "CONST", [128, CO["n"]], F32)
    PPB = Buf(P, st, "PP", [128, L * NPP], F32)
    ident = CONST.t[:, 0:128]
    ones = CONST.t[:, 128:256]
    cinv = CONST.t[:, 256:256 + NGRP * HALO_P].rearrange("p (g c) -> p g c", g=NGRP)
    tri = CONST.t[:, CO["tri"]:CO["tri"] + 128]
    biota = CONST.t[:, CO["biota"]:CO["biota"] + NSB]
    c13 = CONST.t[:, CO["c13"]:CO["c13"] + NG13]
    c2 = CONST.t[:, CO["c2"]:CO["c2"] + 2 * NJG]
    banks = []
    for i in range(8):
        t = st.enter_context(nc.psum_tensor(f"bank{i}", [128, 512], F32))
        banks.append((t, P.res(f"bank{i}", x=True)))
    bank_rr = [0]

    def nb():
        b = banks[bank_rr[0] % 8]
        bank_rr[0] += 1
        return b

    s_init = P.newsem("init")
    s_out = P.newsem("out")
    s_x = [P.newsem(f"ldx{b}") for b in range(NB)]
    dsem_pool = [P.newsem(f"d{i}") for i in range(36)]
    dsem_next = [0]
    hsem_pool = [P.newsem(f"h{i}") for i in range(14)]
    hsem_next = [0]

    def dsem():
        s = dsem_pool[dsem_next[0] % len(dsem_pool)]
        dsem_next[0] += 1
        return s

    def hsem():
        s = hsem_pool[hsem_next[0] % len(hsem_pool)]
        hsem_next[0] += 1
        return s

    P.dma("sp", CONST.t[:], consts_d[:, :], s_init, w=[CONST.r0])
    P.dma("sp", PPB.t[:], pp_d[:, :], s_init, w=[PPB.r0])
    CONST.r0.lw = PPB.r0.lw = (s_init, P.semval[s_init])
    for b in range(NB):
        P.dma("sp", X[:, 4 * b:4 * b + 4, :], x_d[b * 512:(b + 1) * 512, :].rearrange("(t p) d -> p t d", p=128),
              s_x[b], w=Xr[4 * b:4 * b + 4])

    act_copy_toggle = [0]

    def evac(out, in_, r, w, eng=None):
        if eng is None:
            eng = ("act", "dve")[act_copy_toggle[0] % 2]
            act_copy_toggle[0] += 1
        if eng == "act":
            P.op("act", lambda h: h.copy(out=out, in_=in_), r=r, w=w)
        else:
            P.op("dve", lambda h: h.tensor_copy(out=out, in_=in_), r=r, w=w)

    def to_fm(XT, b, lo=None, src=None):
        for tt in range(4):
            t = 4 * b + tt
            bk = [nb(), nb()]
            sap, sres = (X[:, t, :], Xr[t]) if src is None else src(tt)

            def tr(h, sap=sap, bk=bk):
                for kc in range(KC):
                    ins = h.transpose(out=bk[kc // 4][0][:, (kc % 4) * 128:(kc % 4 + 1) * 128],
                                      in_=sap[:, kc * 128:(kc + 1) * 128], identity=ident)
                return ins
            P.op("pe", tr, r=[sres, CONST.r0], w=[bk[0][1], bk[1][1]])
            for hh in range(2):
                o = XT.t[:, 4 * hh:4 * hh + 4, tt * 128:(tt + 1) * 128]
                i = bk[hh][0][:].rearrange("p (k t) -> p k t", k=4)
                pr = XT.r[tt * 2 + hh]
                if lo is None:
                    evac(o, i, r=[bk[hh][1]], w=[pr])
                else:
                    evac(o, i, r=[bk[hh][1]], w=[pr], eng="act")
                    lo_o = lo.t[:, tt, 4 * hh:4 * hh + 4, :]
                    P.op("dve", lambda h, lo_o=lo_o, i=i, o=o: h.tensor_tensor(out=lo_o, in0=i, in1=o, op=ALU.subtract),
                         r=[bk[hh][1], pr], w=[lo.r[tt * 2 + hh]])

    def ln_stats(ph_tmp, Z, Zr):
        ST, MV, SD = ph_tmp
        P.op("dve", lambda h: h.bn_stats(out=ST.t[:, 0, :], in_=Z[:, 0:512]), r=Zr, w=[ST.r0])
        P.op("dve", lambda h: h.bn_stats(out=ST.t[:, 1, :], in_=Z[:, 512:1024]), r=Zr, w=[ST.r0])
        P.op("dve", lambda h: h.bn_aggr(out=MV.t[:], in_=ST.t[:].rearrange("p a b -> p (a b)")), r=[ST.r0], w=[MV.r0])
        P.op("act", lambda h: h.activation(out=SD.t[:, 0:1], in_=MV.t[:, 1:2], func=AF.Sqrt, bias=epsb, scale=1.0),
             r=[MV.r0, EPS.r0], w=[SD.r0])

    def ln_norm(ph_tmp, Z, Zr):
        ST, MV, SD = ph_tmp
        P.op("dve", lambda h: h.reciprocal(out=SD.t[:, 1:2], in_=SD.t[:, 0:1]), r=[SD.r0], w=[SD.r0])
        P.op("dve", lambda h: h.tensor_scalar(out=SD.t[:, 2:3], in0=MV.t[:, 0:1], scalar1=SD.t[:, 1:2], scalar2=-1.0,
                                              op0=ALU.mult, op1=ALU.mult), r=[MV.r0], w=[SD.r0])
        P.op("act", lambda h: h.activation(out=Z, in_=Z, func=AF.Identity, scale=SD.t[:, 1:2], bias=SD.t[:, 2:3]),
             r=[SD.r0], w=Zr)

    def ln_affine(Z, Zr, gb, gbr, out_ap, out_r, extra_r=()):
        P.op("dve", lambda h: h.tensor_tensor(out=Z, in0=Z, in1=gb[0], op=ALU.mult), r=[gbr], w=Zr)
        P.op("dve", lambda h: h.tensor_tensor(out=out_ap, in0=Z, in1=gb[1], op=ALU.add), r=Zr + [gbr] + list(extra_r), w=[out_r])

    def layer_norm(ph_tmp, Z, Zr, gb, gbr, out_ap, out_r, extra_r=()):
        Zr = list(Zr) if isinstance(Zr, (list, tuple)) else [Zr]
        ln_stats(ph_tmp, Z, Zr)
        ln_norm(ph_tmp, Z, Zr)
        ln_affine(Z, Zr, gb, gbr, out_ap, out_r, extra_r)

    EPS = Buf(P, st, "EPS", [128, 1], F32)
    P.op("dve", lambda h: h.memset(EPS.t[:], LN_EPS), w=[EPS.r0])
    epsb = EPS.t[:, 0:1]

    def load_w(q, buf, src, sem, **kw):
        P.dma(q, buf.t[:], src, sem, w=[buf.r0], max_dma_last_dim=8192, **kw)

    def mix_phase(l):
        with ExitStack() as ph:
            WIN = Buf(P, ph, "WIN", [128, KC, DIN], BF16, nres=3)
            WOUT = Buf(P, ph, "WOUT", [128, KC, D], BF16)
            CPW = Buf(P, ph, "CPW", [128, 4, DCONV], BF16)
            PW = Buf(P, ph, "PW", [128, NGRP, 128], BF16)
            LNB = Buf(P, ph, "LNB1", [128, 2, D], F32)
            XT = Buf(P, ph, "XTm", [128, KC, 512], BF16, nres=8)
            U = Buf(P, ph, "U", [128, NGRP, HALO_P + 512], F32, nres=NGRP + 1)
            PA = Buf(P, ph, "PA", [128, HALO_P + 512], F32)
            PB = Buf(P, ph, "PB", [128, HALO_P + 512], F32)
            DP = Buf(P, ph, "DPL", [128, NGRP, 512], BF16, nres=NGRP)
            V = Buf(P, ph, "V", [128, 4, HALO_C + 512], BF16, nres=5)
            DG = [Buf(P, ph, f"DG{i}", [128, CW, 128], BF16) for i in range(4)]
            YCs = [Buf(P, ph, "YC", [128, 4, 512], F32, nres=4)] * 2
            SN = Buf(P, ph, "SN", [128, 4, 512], BF16, nres=4)
            MIP = [Buf(P, ph, "MIP", [128, 4, 512], BF16, nres=4)] * 2
            MIC = Buf(P, ph, "MIC", [128, 4, 512], BF16, nres=4)
            TMP = [Buf(P, ph, f"TMPm{i}", [128, 512], F32) for i in range(2)]
            SQ = TMP
            MEAN = Alias(PA.t[:, 0:512], PA.r)
            VAR = Alias(PB.t[:, 0:512], PB.r)
            RSTD = Buf(P, ph, "RSTD", [128, 512], F32)
            T16 = Buf(P, ph, "T16", [128, HALO_P], F32)
            lnt = [(Buf(P, ph, f"STm{i}", [128, 2, 6], F32), Buf(P, ph, f"MVm{i}", [128, 2], F32),
                    Buf(P, ph, f"SDm{i}", [128, 4], F32)) for i in range(2)]

            for i in range(3):
                P.dma("pool", WIN.t[:, :, i * 512:(i + 1) * 512], w_in_d[l][:, i * 512:(i + 1) * 512].rearrange("(k p) f -> p k f", p=128),
                      dsem(), w=[WIN.r[i]], max_dma_last_dim=8192)
            load_w("pool", PW, pool_w_d[l].rearrange("g c d -> c g d"), dsem())
            load_w("pool", CPW, conv_pw_d[l].rearrange("(k p) f -> p k f", p=128), dsem())
            load_w("pool", WOUT, w_out_d[l].rearrange("(k p) f -> p k f", p=128), dsem())
            P.dma("sp", LNB.t[:], lnb_d[4 * l:4 * l + 2, :].partition_broadcast(128), hsem(), w=[LNB.r0])
            pp0 = l * NPP
            P.op("dve", lambda h: h.memset(U.t[:, :, 0:HALO_P], 0.0), w=U.r)
            P.op("dve", lambda h: h.memset(V.t[:, :, 0:HALO_C], 0.0), w=V.r)
            W = HALO_P + 512
            for c in range(4):
                def mk(h, c=c):
                    for k in range(CW):
                        col = pp0 + 16 + c * CW + k
                        ins = h.tensor_scalar(out=DG[c].t[:, k, :], in0=ident, scalar1=PPB.t[:, col:col + 1], scalar2=None,
                                              op0=ALU.mult)
                    return ins
                P.op("dve", mk, r=[CONST.r0, PPB.r0], w=[DG[c].r0])

            def inproj(oc, bk):
                def f(h):
                    for kc in range(KC):
                        ins = h.matmul(bk[0][:], lhsT=WIN.t[:, kc, oc * 128:(oc + 1) * 128], rhs=XT.t[:, kc, :],
                                       start=(kc == 0), stop=(kc == KC - 1))
                    return ins
                P.op("pe", f, r=[WIN.r[oc // 4]] + XT.r, w=[bk[1]])

            def S0(b):
                to_fm(XT, b)

            def S1(b):
                for g in range(NGRP):
                    bk = nb()
                    inproj(g, bk)
                    evac(U.t[:, g, HALO_P:], bk[0][:], r=[bk[1]], w=[U.r[g]])
                for c in range(4):
                    bv, bg = nb(), nb()
                    inproj(4 + c, bv)
                    inproj(8 + c, bg)
                    tmp = TMP[c % 2]
                    P.op("act", lambda h, bg=bg, tmp=tmp: h.activation(out=tmp.t[:], in_=bg[0][:], func=AF.Sigmoid),
                         r=[bg[1]], w=[tmp.r0])
                    P.op("dve", lambda h, bv=bv, tmp=tmp, c=c: h.tensor_tensor(out=V.t[:, c, HALO_C:], in0=bv[0][:], in1=tmp.t[:],
                                                                                op=ALU.mult),
                         r=[bv[1], tmp.r0], w=[V.r[c]])

            def S2p(b):
                MI = MIP[b % 2]
                for g in range(NGRP):
                    src, srcr = U.t[:, g, :], U.r[g]
                    pp = [PA, PB]
                    k, lo = 1, 0
                    for step in range(g + 1):
                        dst = pp[step % 2]
                        P.op("dve", lambda h, dst=dst, src=src, k=k, lo=lo: h.tensor_tensor(
                            out=dst.t[:, lo + k:W], in0=src[:, lo + k:W], in1=src[:, lo:W - k], op=ALU.add),
                             r=[srcr], w=[dst.r0])
                        src, srcr = dst.t[:], dst.r0
                        lo += k
                        k *= 2
                    w = WINS[g]
                    P.op("dve", lambda h, src=src, g=g, w=w: h.scalar_tensor_tensor(out=DP.t[:, g, :], in0=src[:, HALO_P:W],
                                                                                    scalar=1.0 / w, in1=U.t[:, g, HALO_P:W],
                                                                                    op0=ALU.mult, op1=ALU.subtract),
                         r=[srcr, U.r[g]], w=[DP.r[g]])
                    if b == 0:
                        P.op("dve", lambda h, src=src, g=g: h.tensor_tensor(out=T16.t[:], in0=src[:, HALO_P:2 * HALO_P],
                                                                            in1=cinv[:, g, :], op=ALU.mult),
                             r=[srcr, CONST.r0], w=[T16.r0])
                        P.op("dve", lambda h, g=g: h.tensor_tensor(out=DP.t[:, g, 0:HALO_P], in0=T16.t[:],
                                                                   in1=U.t[:, g, HALO_P:2 * HALO_P], op=ALU.subtract),
                             r=[T16.r0, U.r[g]], w=[DP.r[g]])
                    bk = nb()
                    P.op("pe", lambda h, g=g, bk=bk: h.matmul(bk[0][:], lhsT=PW.t[:, g, :], rhs=DP.t[:, g, :], start=True, stop=True),
                         r=[PW.r0, DP.r[g]], w=[bk[1]])
                    P.op("act", lambda h, g=g, bk=bk: h.activation(out=MI.t[:, g, :], in_=bk[0][:], func=AF.Identity,
                                                                   scale=PPB.t[:, pp0 + g:pp0 + g + 1]),
                         r=[bk[1], PPB.r0], w=[MI.r[g]])
                if b + 1 < NB:
                    P.op("dve", lambda h: h.tensor_copy(out=U.t[:, :, 0:HALO_P], in_=U.t[:, :, 512:512 + HALO_P]),
                         r=[], w=U.r)

            def S2c(b):
                YC = YCs[b % 2]
                for c in range(4):
                    dg = DG[c]
                    bk = nb()

                    def cv(h, c=c, dg=dg, bk=bk):
                        for k in range(CW):
                            ins = h.matmul(bk[0][:], lhsT=dg.t[:, k, :], rhs=V.t[:, c, 2 + k:2 + k + 512],
                                           start=(k == 0), stop=(k == CW - 1))
                        return ins
                    P.op("pe", cv, r=[dg.r0, V.r[c]], w=[bk[1]])
                    P.op("act", lambda h, c=c, bk=bk: h.activation(out=YC.t[:, c, :], in_=bk[0][:], func=AF.Identity,
                                                                   bias=PPB.t[:, pp0 + 4 + c:pp0 + 5 + c], scale=1.0),
                         r=[bk[1], PPB.r0], w=[YC.r[c]])
                if b + 1 < NB:
                    P.op("dve", lambda h: h.tensor_copy(out=V.t[:, :, 0:HALO_C], in_=V.t[:, :, 512:512 + HALO_C]),
                         r=[], w=V.r)

            def S3(b):
                YC = YCs[b % 2]
                b1, b2 = nb(), nb()

                def s1(h, b1=b1):
                    for c in range(4):
                        ins = h.matmul(b1[0][:], lhsT=ones, rhs=YC.t[:, c, :], start=(c == 0), stop=(c == 3))
                    return ins
                P.op("pe", s1, r=[CONST.r0] + YC.r, w=[b1[1]])
                for c in range(4):
                    sq = SQ[c % 2]
                    P.op("dve", lambda h, c=c, sq=sq: h.tensor_tensor(out=sq.t[:], in0=YC.t[:, c, :], in1=YC.t[:, c, :], op=ALU.mult),
                         r=[YC.r[c]], w=[sq.r0])
                    P.op("pe", lambda h, c=c, sq=sq, b2=b2: h.matmul(b2[0][:], lhsT=ones, rhs=sq.t[:], start=(c == 0), stop=(c == 3)),
                         r=[CONST.r0, sq.r0], w=[b2[1]])
                P.op("act", lambda h, b1=b1: h.mul(out=MEAN.t[:], in_=b1[0][:], mul=1.0 / DCONV), r=[b1[1]], w=[MEAN.r0])
                P.op("dve", lambda h: h.tensor_tensor(out=VAR.t[:], in0=MEAN.t[:], in1=MEAN.t[:], op=ALU.mult),
                     r=[MEAN.r0], w=[VAR.r0])
                P.op("dve", lambda h, b2=b2: h.scalar_tensor_tensor(out=VAR.t[:], in0=b2[0][:], scalar=1.0 / DCONV, in1=VAR.t[:],
                                                                   op0=ALU.mult, op1=ALU.subtract),
                     r=[b2[1]], w=[VAR.r0])
                P.op("act", lambda h: h.activation(out=VAR.t[:], in_=VAR.t[:], func=AF.Sqrt, bias=epsb, scale=1.0),
                     r=[EPS.r0], w=[VAR.r0])
                P.op("dve", lambda h: h.reciprocal(out=RSTD.t[:], in_=VAR.t[:]), r=[VAR.r0], w=[RSTD.r0])

            def S4(b):
                YC = YCs[b % 2]
                for c in range(4):
                    tmp = TMP[c % 2]
                    P.op("dve", lambda h, c=c, tmp=tmp: h.tensor_tensor(out=tmp.t[:], in0=YC.t[:, c, :], in1=MEAN.t[:], op=ALU.subtract),
                         r=[YC.r[c], MEAN.r0], w=[tmp.r0])
                    P.op("dve", lambda h, tmp=tmp: h.tensor_tensor(out=tmp.t[:], in0=tmp.t[:], in1=RSTD.t[:], op=ALU.mult),
                         r=[RSTD.r0], w=[tmp.r0])
                    P.op("act", lambda h, c=c, tmp=tmp: h.activation(out=SN.t[:, c, :], in_=tmp.t[:], func=AF.Silu,
                                                                     scale=PPB.t[:, pp0 + 8 + c:pp0 + 9 + c],
                                                                     bias=PPB.t[:, pp0 + 12 + c:pp0 + 13 + c]),
                         r=[tmp.r0, PPB.r0], w=[SN.r[c]])

            def S4p(b):
                for oc in range(4):
                    bk = nb()

                    def pw(h, oc=oc, bk=bk):
                        for kc in range(4):
                            ins = h.matmul(bk[0][:], lhsT=CPW.t[:, kc, oc * 128:(oc + 1) * 128], rhs=SN.t[:, kc, :],
                                           start=(kc == 0), stop=(kc == 3))
                        return ins
                    P.op("pe", pw, r=[CPW.r0] + SN.r, w=[bk[1]])
                    evac(MIC.t[:, oc, :], bk[0][:], r=[bk[1]], w=[MIC.r[oc]])

            def S5(b):
                MI = MIP[b % 2]
                YC = YCs[b % 2]
                Z = [Alias(YC.t[:, 2 * i:2 * i + 2, :].rearrange("p a b -> p (a b)"), YC.r[2 * i:2 * i + 2]) for i in range(2)]
                gb = (LNB.t[:, 0, :], LNB.t[:, 1, :])

                def A(tt):
                    t = 4 * b + tt
                    z = Z[tt % 2]
                    for hh in range(2):
                        bk = nb()

                        def wo(h, tt=tt, hh=hh, bk=bk):
                            for kc in range(KC):
                                src = MI.t[:, kc, tt * 128:(tt + 1) * 128] if kc < 4 else MIC.t[:, kc - 4, tt * 128:(tt + 1) * 128]
                                ins = h.matmul(bk[0][:], lhsT=src, rhs=WOUT.t[:, kc, hh * 512:(hh + 1) * 512],
                                               start=(kc == 0), stop=(kc == KC - 1))
                            return ins
                        P.op("pe", wo, r=[WOUT.r0] + MI.r + MIC.r, w=[bk[1]])
                        P.op("dve", lambda h, t=t, hh=hh, bk=bk, z=z: h.scalar_tensor_tensor(
                            out=z.t[:, hh * 512:(hh + 1) * 512], in0=X[:, t, hh * 512:(hh + 1) * 512], scalar=alpha,
                            in1=bk[0][:], op0=ALU.mult, op1=ALU.add), r=[bk[1], Xr[t]], w=z.r)
                    ln_stats(lnt[tt % 2], z.t[:], list(z.r))

                def B(tt):
                    z = Z[tt % 2]
                    ln_norm(lnt[tt % 2], z.t[:], list(z.r))

                def C(tt):
                    t = 4 * b + tt
                    z = Z[tt % 2]
                    ln_affine(z.t[:], list(z.r), gb, LNB.r0, X[:, t, :], Xr[t])
                for step in (A, 0), (A, 1), (B, 0), (B, 1), (C, 0), (A, 2), (C, 1), (A, 3), (B, 2), (B, 3), (C, 2), (C, 3):
                    step[0](step[1])

            S0(0)
            S1(0)
            S2c(0)
            S2p(0)
            for b in range(NB):
                if b + 1 < NB:
                    S0(b + 1)
                S3(b)
                S4(b)
                if b + 1 < NB:
                    S1(b + 1)
                S4p(b)
                S5(b)
                if b + 1 < NB:
                    S2c(b + 1)
                    S2p(b + 1)
            P.barrier()

    def ffn_block(ph, XT, ld13, ld2, FF, rings, G, consume, mid=None, mid2=None):
        W1S, W3S, W2S = rings
        NJ = FF // 128
        for g in range(FF // 256):
            s1 = W1S[g % len(W1S)]
            s3 = W3S[g % len(W3S)]
            ld13(0, g, s1)
            ld13(1, g, s3)
            for jj in range(2):
                j = 2 * g + jj
                bk1, bk3 = nb(), nb()
                for (ws, bk) in ((s1, bk1), (s3, bk3)):
                    def f(h, ws=ws, bk=bk, jj=jj):
                        for kc in range(KC):
                            ins = h.matmul(bk[0][:], lhsT=ws[0].t[:, kc, jj * 128:(jj + 1) * 128], rhs=XT.t[:, kc, :],
                                           start=(kc == 0), stop=(kc == KC - 1))
                        return ins
                    P.op("pe", f, r=[ws[0].r0] + XT.r, w=[bk[1]])
                tmp = ph["SIL"][j % 2]
                P.op("act", lambda h, bk1=bk1, tmp=tmp: h.activation(out=tmp.t[:], in_=bk1[0][:], func=AF.Silu),
                     r=[bk1[1]], w=[tmp.r0])
                P.op("dve", lambda h, bk3=bk3, tmp=tmp, j=j: h.tensor_tensor(out=G.t[:, j, :], in0=bk3[0][:], in1=tmp.t[:], op=ALU.mult),
                     r=[bk3[1], tmp.r0], w=[G.r[j]])
        if mid is not None:
            mid()
        bounds = list(range(0, NJ, 4)) + [NJ]
        ngr = len(bounds) - 1
        for hh in range(2):
            yb = [nb() for _ in range(4)]
            for gi in range(ngr):
                j0, j1 = bounds[gi], bounds[gi + 1]
                ws = W2S[(hh * ngr + gi) % len(W2S)]
                ld2(hh, gi, j0, j1, ws)
                for tt in range(4):
                    def f(h, tt=tt, j0=j0, j1=j1, ws=ws):
                        for j in range(j0, j1):
                            ins = h.matmul(yb[tt][0][:], lhsT=G.t[:, j, tt * 128:(tt + 1) * 128], rhs=ws[0].t[:, j - j0, :],
                                           start=(j == 0), stop=(j == NJ - 1))
                        return ins
                    P.op("pe", f, r=[ws[0].r0] + G.r[j0:j1], w=[yb[tt][1]])
            for tt in range(4):
                consume(tt, hh, yb[tt])
            if hh == 0 and mid2 is not None:
                mid2()

    def plain_loaders(w1, w3, w2nat):
        def ld13(which, g, slot):
            src = (w1, w3)[which][g].rearrange("p (k f) -> p k f", k=KC)
            P.dma("pool", slot[0].t[:], src, slot[1], w=[slot[0].r0], max_dma_last_dim=8192)

        def ld2(hh, gi, j0, j1, slot):
            src = w2nat[j0 * 128:j1 * 128, hh * 512:(hh + 1) * 512].rearrange("(j p) d -> p j d", p=128)
            P.dma("pool", slot[0].t[:, 0:j1 - j0, :], src, slot[1], w=[slot[0].r0], max_dma_last_dim=8192)
        return ld13, ld2

    def make_rings(ph, stack):
        W1S = [(Buf(P, stack, f"W1S{i}", [128, KC, 256], BF16), dsem()) for i in range(3)]
        W3S = [(Buf(P, stack, f"W3S{i}", [128, KC, 256], BF16), dsem()) for i in range(3)]
        W2S = [(Buf(P, stack, f"W2S{i}", [128, 4, 512], BF16), dsem()) for i in range(4)]
        ph["SIL"] = [Buf(P, stack, f"SIL{i}", [128, 512], F32) for i in range(2)]
        return (W1S, W3S, W2S)

    def dense_phase(l):
        j = l // 2
        with ExitStack() as stack:
            ph = {}
            rings = make_rings(ph, stack)
            NJ = FFD // 128
            G = Buf(P, stack, "Gd", [128, NJ, 512], BF16, nres=NJ)
            XTs = [Buf(P, stack, f"XTf{i}", [128, KC, 512], BF16, nres=8) for i in range(2)]
            LNB = Buf(P, stack, "LNB2", [128, 2, D], F32)
            Z = Buf(P, stack, "Zf", [128, 4, D], F32, nres=4)
            lnt = [(Buf(P, stack, f"STf{i}", [128, 2, 6], F32), Buf(P, stack, f"MVf{i}", [128, 2], F32),
                    Buf(P, stack, f"SDf{i}", [128, 4], F32)) for i in range(4)]
            P.dma("sp", LNB.t[:], lnb_d[4 * l + 2:4 * l + 4, :].partition_broadcast(128), hsem(), w=[LNB.r0])
            ple_emit, ple_loads, ple1, ple2 = ple_parts(stack, l, nxt=1)
            last_layer = (l == L - 1)
            to_fm(XTs[0], 0)
            for b in range(NB):
                XT = XTs[b % 2]

                def mid(b=b):
                    if b + 1 < NB:
                        to_fm(XTs[(b + 1) % 2], b + 1)
                    if b == 0:
                        ple_loads()
                    if b >= 1:
                        ple1(b - 1)

                def mid2(b=b):
                    if b >= 1:
                        ple2(b - 1, last_layer)

                def consume(tt, hh, bk, b=b):
                    t = 4 * b + tt
                    P.op("dve", lambda h: h.scalar_tensor_tensor(
                        out=Z.t[:, tt, hh * 512:(hh + 1) * 512], in0=X[:, t, hh * 512:(hh + 1) * 512], scalar=alpha,
                        in1=bk[0][:], op0=ALU.mult, op1=ALU.add), r=[bk[1], Xr[t]], w=[Z.r[tt]])
                    if hh == 1:
                        gb = (LNB.t[:, 0, :], LNB.t[:, 1, :])
                        zt = lambda i: Z.t[:, i, :]
                        aff = lambda i: ln_affine(zt(i), [Z.r[i]], gb, LNB.r0, X[:, 4 * b + i, :], Xr[4 * b + i])
                        ln_stats(lnt[tt], zt(tt), [Z.r[tt]])
                        if tt >= 1:
                            ln_norm(lnt[tt - 1], zt(tt - 1), [Z.r[tt - 1]])
                        if tt >= 2:
                            aff(tt - 2)
                        if tt == 3:
                            ln_norm(lnt[3], zt(3), [Z.r[3]])
                            aff(2)
                            aff(3)
                ld13, ld2 = plain_loaders(dw1_d[j], dw3_d[j], dw2_d[j])
                ffn_block(ph, XT, ld13, ld2, FFD, rings, G, consume, mid=mid, mid2=mid2)
            ple_emit(NB - 1, last_layer)
            P.barrier()

    def moe_phase(l):
        jm = l // 2
        NJ = FFE // 128
        NC4 = NSB * 4
        AX = mybir.AxisListType.X
        with ExitStack() as outer:
            LG = Buf(P, outer, "LG", [128, NT, E], F32, nres=NT)
            M8 = Buf(P, outer, "M8", [128, NT, 8], F32, nres=NT)
            MK = Buf(P, outer, "MK", [128, NT, E], F32, nres=NT)
            OH1 = Buf(P, outer, "OH1", [128, NT, E], F32, nres=NT)
            GT = Buf(P, outer, "GT", [128, NT, E], F32, nres=NT)
            SM = Buf(P, outer, "SM", [128, NT, 4], F32, nres=NT)
            SL1I = Buf(P, outer, "SL1I", [128, NT], I32)
            SL2I = Buf(P, outer, "SL2I", [128, NT], I32)
            G12 = Buf(P, outer, "G12", [128, 2, NT], F32)
            GIDX = Buf(P, outer, "GIDX", [128, NC4], I32)
            IDX13 = Buf(P, outer, "IDX13", [128, NSB, NG13], I32)
            IDX2 = Buf(P, outer, "IDX2", [128, NSB, 2 * NJG], I32)
            TOKID = Buf(P, outer, "TOKID", [128, NT], I32)
            NBTI = Buf(P, outer, "NBTI", [128, 1], I32)
            JUNK = Buf(P, outer, "JUNK", [128, 64], F32)
            ZT = Buf(P, outer, "ZT", [128, D], F32)
            P.op("dve", lambda h: h.memset(ZT.t[:], 0.0), w=[ZT.r0])
            LNB = Buf(P, outer, "LNB2e", [128, 2, D], F32)
            X1Dr = [P.res(f"x1d{t}") for t in range(NT)]
            SMr = P.res("slotmap")
            YDr = [P.res(f"yd{b}") for b in range(NSB)]
            s_x1d, s_sm = hsem(), hsem()
            s_yd = [hsem() for _ in range(4)]
            P.dma("sp", LNB.t[:], lnb_d[4 * l + 2:4 * l + 4, :].partition_broadcast(128), hsem(), w=[LNB.r0])
            P.dma("sp", TOKID.t[:], tokid_d[:, :], hsem(), w=[TOKID.r0])
            with ExitStack() as stack:
                XT = Buf(P, stack, "XTr", [128, KC, 512], BF16, nres=8)
                LO = Buf(P, stack, "XLO", [128, 4, KC, 128], BF16, nres=8)
                RW = Buf(P, stack, "RW", [128, KC, E], F32)
                RWH = Buf(P, stack, "RWH", [128, KC, E], BF16)
                RWL = Buf(P, stack, "RWL", [128, KC, E], BF16)
                CS = Buf(P, stack, "CS", [128, NT, E], F32)
                WS = Buf(P, stack, "WS", [128, NT, E], F32)
                OFF = Buf(P, stack, "OFF", [128, NT, E], F32)
                SLOT = Buf(P, stack, "SLOT", [128, NT, E], F32)
                PR = Buf(P, stack, "PR", [128, NT, E], F32)
                OH2 = Buf(P, stack, "OH2", [128, NT, E], F32)
                SV = Buf(P, stack, "SV", [128, 8, E], F32)
                DEN = Buf(P, stack, "DEN", [128, NT, 1], F32)
                SLF = Buf(P, stack, "SLF", [128, 2, NT], F32)
                EB = Buf(P, stack, "EB", [128, 3, NSB], F32)
                I13F = Buf(P, stack, "I13F", [128, NSB, NG13], F32)
                I2F = Buf(P, stack, "I2F", [128, NSB, 2 * NJG], F32)
                ZI = Buf(P, stack, "ZI", [128, NC4], I32)
                SMT = Buf(P, stack, "SMT", [NC4, 128], I32)
                SMTF = Buf(P, stack, "SMTF", [NC4, 128], F32)
                P.dma("sp", RW.t[:], router_d[jm].rearrange("(k p) e -> p k e", p=128), hsem(), w=[RW.r0])
                P.op("dve", lambda h: h.tensor_copy(out=RWH.t[:], in_=RW.t[:]), r=[RW.r0], w=[RWH.r0])
                P.op("dve", lambda h: h.tensor_tensor(out=RWL.t[:], in0=RW.t[:], in1=RWH.t[:], op=ALU.subtract),
                     r=[RW.r0, RWH.r0], w=[RWL.r0])
                P.op("dve", lambda h: h.memset(ZI.t[:], 0), w=[ZI.r0])
                P.dma("sp", SLOTMAP.rearrange("(p c) o -> p (c o)", p=128), ZI.t[:], s_sm, r=[ZI.r0], w=[SMr])
                for b in range(NB):
                    to_fm(XT, b, lo=LO)
                    for tt in range(4):
                        t = 4 * b + tt
                        P.dma("sp", X1D[t * 128:(t + 1) * 128, :], X[:, t, :], s_x1d, r=[Xr[t]], w=[X1Dr[t]])
                        bk = nb()

                        def rt(h, tt=tt, bk=bk):
                            n = 0
                            for kc in range(KC):
                                hi = XT.t[:, kc, tt * 128:(tt + 1) * 128]
                                lo = LO.t[:, tt, kc, :]
                                for (a, wv) in ((hi, RWH), (lo, RWH), (hi, RWL)):
                                    ins = h.matmul(bk[0][:, 0:E], lhsT=a, rhs=wv.t[:, kc, :], start=(n == 0), stop=(n == 3 * KC - 1))
                                    n += 1
                            return ins
                        P.op("pe", rt, r=[RWH.r0, RWL.r0, XT.r[2 * tt], XT.r[2 * tt + 1], LO.r[2 * tt], LO.r[2 * tt + 1]], w=[bk[1]])
                        lg, m8, mk, gt, sm, oh1 = LG.t[:, t, :], M8.t[:, t, :], MK.t[:, t, :], GT.t[:, t, :], SM.t[:, t, :], OH1.t[:, t, :]
                        P.op("dve", lambda h, bk=bk, lg=lg: h.tensor_copy(out=lg, in_=bk[0][:, 0:E]), r=[bk[1]], w=[LG.r[t]])
                        P.op("dve", lambda h, lg=lg, m8=m8: h.max(out=m8, in_=lg), r=[LG.r[t]], w=[M8.r[t]])
                bc = lambda ap: ap.to_broadcast([128, NT, E])
                m1, m2 = M8.t[:, :, 0:1], M8.t[:, :, 1:2]
                P.op("dve", lambda h: h.tensor_tensor(out=MK.t[:], in0=LG.t[:], in1=bc(m2), op=ALU.is_ge), r=LG.r + M8.r, w=MK.r)
                P.op("dve", lambda h: h.tensor_tensor(out=OH1.t[:], in0=LG.t[:], in1=bc(m1), op=ALU.is_equal), r=LG.r + M8.r, w=OH1.r)
                P.op("dve", lambda h: h.tensor_tensor(out=GT.t[:], in0=LG.t[:], in1=bc(m1), op=ALU.subtract), r=LG.r + M8.r, w=GT.r)
                P.op("act", lambda h: h.activation(out=GT.t[:], in_=GT.t[:], func=AF.Exp), w=GT.r)
                P.op("dve", lambda h: h.tensor_tensor(out=GT.t[:], in0=GT.t[:], in1=MK.t[:], op=ALU.mult), r=MK.r, w=GT.r)
                P.op("dve", lambda h: h.reduce_sum(out=DEN.t[:].rearrange("p t o -> p (t o)"), in_=GT.t[:], axis=AX), r=GT.r, w=[DEN.r0])
                P.op("dve", lambda h: h.reciprocal(out=DEN.t[:], in_=DEN.t[:]), w=[DEN.r0])
                P.op("dve", lambda h: h.tensor_tensor(out=GT.t[:], in0=GT.t[:], in1=bc(DEN.t[:, :, 0:1]), op=ALU.mult), r=[DEN.r0], w=GT.r)
                fl = lambda buf: buf.t[:].rearrange("p t e -> p (t e)")
                bC, bW = nb(), nb()
                P.op("pe", lambda h: h.matmul(bC[0][:, 0:NT * E], lhsT=ones, rhs=fl(MK), start=True, stop=True), r=[CONST.r0] + MK.r, w=[bC[1]])
                P.op("pe", lambda h: h.matmul(bW[0][:, 0:NT * E], lhsT=tri, rhs=fl(MK), start=True, stop=True), r=[CONST.r0] + MK.r, w=[bW[1]])
                P.op("dve", lambda h: h.tensor_copy(out=fl(CS), in_=bC[0][:, 0:NT * E]), r=[bC[1]], w=[CS.r0])
                P.op("dve", lambda h: h.tensor_copy(out=fl(WS), in_=bW[0][:, 0:NT * E]), r=[bW[1]], w=[WS.r0])
                P.op("dve", lambda h: h.memset(OFF.t[:, 0, :], 0.0), w=[OFF.r0])
                for t in range(1, NT):
                    P.op("dve", lambda h, t=t: h.tensor_tensor(out=OFF.t[:, t, :], in0=OFF.t[:, t - 1, :], in1=CS.t[:, t - 1, :], op=ALU.add),
                         r=[CS.r0], w=[OFF.r0])
                cnt, nbk, end, b5 = SV.t[:, 0, :], SV.t[:, 1, :], SV.t[:, 2, :], SV.t[:, 3, :]
                P.op("dve", lambda h: h.tensor_tensor(out=cnt, in0=OFF.t[:, NT - 1, :], in1=CS.t[:, NT - 1, :], op=ALU.add),
                     r=[OFF.r0, CS.r0], w=[SV.r0])
                P.op("dve", lambda h: h.tensor_scalar(out=nbk, in0=cnt, scalar1=0.0, scalar2=None, op0=ALU.is_gt), w=[SV.r0])
                for j in range(1, NB):
                    P.op("dve", lambda h, j=j: h.scalar_tensor_tensor(out=nbk, in0=cnt, scalar=512.0 * j, in1=nbk, op0=ALU.is_gt, op1=ALU.add),
                         w=[SV.r0])
                P.op("dve", lambda h: h.tensor_copy(out=end[:, 0:1], in_=nbk[:, 0:1]), w=[SV.r0])
                for e in range(1, E):
                    P.op("dve", lambda h, e=e: h.tensor_tensor(out=end[:, e:e + 1], in0=end[:, e - 1:e], in1=nbk[:, e:e + 1], op=ALU.add),
                         w=[SV.r0])
                P.op("dve", lambda h: h.tensor_copy(out=NBTI.t[:], in_=end[:, E - 1:E]), r=[SV.r0], w=[NBTI.r0])
                P.op("dve", lambda h: h.tensor_tensor(out=b5, in0=end, in1=nbk, op=ALU.subtract), w=[SV.r0])
                P.op("dve", lambda h: h.tensor_scalar(out=b5, in0=b5, scalar1=512.0, scalar2=-1.0, op0=ALU.mult, op1=ALU.add), w=[SV.r0])
                P.op("dve", lambda h: h.tensor_tensor(out=fl(SLOT), in0=fl(OFF), in1=fl(WS), op=ALU.add), r=[OFF.r0, WS.r0], w=[SLOT.r0])
                for t in range(NT):
                    P.op("dve", lambda h, t=t: h.tensor_tensor(out=SLOT.t[:, t, :], in0=SLOT.t[:, t, :], in1=b5, op=ALU.add),
                         r=[SV.r0], w=[SLOT.r0])
                P.op("dve", lambda h: h.tensor_tensor(out=fl(OH2), in0=fl(MK), in1=fl(OH1), op=ALU.subtract), r=MK.r + OH1.r, w=[OH2.r0])
                for (oh, ohr, other, otherr, dst) in ((OH1, OH1.r, SLOT, [SLOT.r0], SLF.t[:, 0, :]), (OH2, [OH2.r0], SLOT, [SLOT.r0], SLF.t[:, 1, :]),
                                                      (OH1, OH1.r, GT, GT.r, G12.t[:, 0, :]), (OH2, [OH2.r0], GT, GT.r, G12.t[:, 1, :])):
                    dres = SLF.r0 if other is SLOT else G12.r0
                    P.op("dve", lambda h, oh=oh, other=other: h.tensor_tensor(out=fl(PR), in0=fl(oh), in1=fl(other), op=ALU.mult),
                         r=list(ohr) + list(otherr), w=[PR.r0])
                    P.op("dve", lambda h, dst=dst: h.reduce_sum(out=dst, in_=PR.t[:], axis=AX), r=[PR.r0], w=[dres])
                P.op("dve", lambda h: h.tensor_copy(out=SL1I.t[:], in_=SLF.t[:, 0, :]), r=[SLF.r0], w=[SL1I.r0])
                P.op("dve", lambda h: h.tensor_copy(out=SL2I.t[:], in_=SLF.t[:, 1, :]), r=[SLF.r0], w=[SL2I.r0])
                eb, eb13, eb2 = EB.t[:, 0, :], EB.t[:, 1, :], EB.t[:, 2, :]
                P.op("dve", lambda h: h.memset(eb, 0.0), w=[EB.r0])
                for e in range(E):
                    P.op("dve", lambda h, e=e: h.scalar_tensor_tensor(out=eb, in0=biota, scalar=end[:, e:e + 1], in1=eb, op0=ALU.is_ge, op1=ALU.add),
                         r=[SV.r0, CONST.r0], w=[EB.r0])
                P.op("dve", lambda h: h.tensor_scalar(out=eb, in0=eb, scalar1=float(E - 1), scalar2=None, op0=ALU.min), w=[EB.r0])
                P.op("dve", lambda h: h.tensor_scalar(out=eb13, in0=eb, scalar1=float(NG13 * 128), scalar2=float(jm * E * NG13 * 128),
                                                      op0=ALU.mult, op1=ALU.add), w=[EB.r0])
                P.op("dve", lambda h: h.tensor_scalar(out=eb2, in0=eb, scalar1=float(2 * NJG * 128), scalar2=float(jm * E * 2 * NJG * 128),
                                                      op0=ALU.mult, op1=ALU.add), w=[EB.r0])
                for b in range(NSB):
                    P.op("dve", lambda h, b=b: h.tensor_scalar(out=I13F.t[:, b, :], in0=c13, scalar1=eb13[:, b:b + 1], scalar2=None, op0=ALU.add),
                         r=[EB.r0, CONST.r0], w=[I13F.r0])
                    P.op("dve", lambda h, b=b: h.tensor_scalar(out=I2F.t[:, b, :], in0=c2, scalar1=eb2[:, b:b + 1], scalar2=None, op0=ALU.add),
                         r=[EB.r0, CONST.r0], w=[I2F.r0])
                P.op("dve", lambda h: h.tensor_copy(out=IDX13.t[:], in_=I13F.t[:]), r=[I13F.r0], w=[IDX13.r0])
                P.op("dve", lambda h: h.tensor_copy(out=IDX2.t[:], in_=I2F.t[:]), r=[I2F.r0], w=[IDX2.r0])
                s_sc = dsem()
                for t in range(NT):
                    for sl in (SL1I, SL2I):
                        P.idma(SLOTMAP[:, :], TOKID.t[:, t:t + 1], s_sc, out_off=sl.t[:, t:t + 1], r=[sl.r0, TOKID.r0, SMr], w=[])
                SMr.lw = (s_sc, P.semval[s_sc])
                P.dma("sp", SMT.t[:], SLOTMAP.rearrange("(c p) o -> c (p o)", p=128), hsem(), r=[SMr], w=[SMT.r0])
                P.op("dve", lambda h: h.tensor_copy(out=SMTF.t[:], in_=SMT.t[:]), r=[SMT.r0], w=[SMTF.r0])
                bT = nb()
                P.op("pe", lambda h: h.transpose(out=bT[0][:, 0:NC4], in_=SMTF.t[:], identity=CONST.t[0:NC4, 0:NC4]),
                     r=[SMTF.r0, CONST.r0], w=[bT[1]])
                P.op("dve", lambda h: h.tensor_copy(out=GIDX.t[:], in_=bT[0][:, 0:NC4]), r=[bT[1]], w=[GIDX.r0])
                P.barrier()
            with ExitStack() as stack:
                ph = {}
                rings = make_rings(ph, stack)
                G = Buf(P, stack, "Ge", [128, NJ, 512], BF16, nres=NJ)
                XG = Buf(P, stack, "XG", [128, 4, D], F32, nres=4)
                xg_sem = [dsem() for _ in range(4)]
                XTs = [Buf(P, stack, f"XTe{i}", [128, KC, 512], BF16, nres=8) for i in range(2)]
                YS = Buf(P, stack, "YS", [128, 4, D], F32, nres=4)
                nbt = nc.values_load(NBTI.t[0:1, 0:1])
                retired = frozenset([sl[1] for ring in rings for sl in ring] + list(xg_sem))
                NB_MIN = (2 * S) // 512

                def prep(b):
                    for tt in range(4):
                        P.idma(XG.t[:, tt, :], X1D[:, :], xg_sem[tt], in_off=GIDX.t[:, 4 * b + tt:4 * b + tt + 1], r=[GIDX.r0], w=[XG.r[tt]])
                    to_fm(XTs[b % 2], b, src=lambda tt: (XG.t[:, tt, :], XG.r[tt]))

                def do_block(b):
                    XT = XTs[b % 2]
                    if b == 0:
                        prep(0)
                    mid = (lambda b=b: prep(b + 1)) if b + 1 < NSB else None

                    def ld13(which, g, slot, b=b):
                        P.idma(slot[0].t[:].rearrange("p k f -> p (k f)"), (ew1_d, ew3_d)[which][:, :], slot[1],
                               in_off=IDX13.t[:, b, g:g + 1], r=[IDX13.r0], w=[slot[0].r0])

                    def ld2(hh, gi, j0, j1, slot, b=b):
                        P.idma(slot[0].t[:].rearrange("p j d -> p (j d)"), ew2_d[:, :], slot[1],
                               in_off=IDX2.t[:, b, hh * NJG + gi:hh * NJG + gi + 1], r=[IDX2.r0], w=[slot[0].r0])

                    def consume(tt, hh, bk, b=b):
                        evac(YS.t[:, tt, hh * 512:(hh + 1) * 512], bk[0][:], r=[bk[1]], w=[YS.r[tt]])
                        if hh == 1:
                            r0 = (4 * b + tt) * 128
                            P.dma("sp", YD[r0:r0 + 128, :], YS.t[:, tt, :], s_yd[tt], r=[YS.r[tt]], w=[])
                    ffn_block(ph, XT, ld13, ld2, FFE, rings, G, consume, mid=mid)
                for b in range(NSB):
                    if b < NB_MIN:
                        do_block(b)
                    else:
                        def fix(eng, si, delta, b=b):
                            if si in s_yd:
                                r0 = (4 * b + s_yd.index(si)) * 128
                                eng.h.dma_start(out=YD[r0:r0 + 128, :], in_=ZT.t[:]).then_inc(P.sems[si], delta)
                                return True
                            return False
                        P.cond(nbt > b, lambda b=b: do_block(b), JUNK.t[:], junk_d[:, :], fix=fix, retired=retired)
                P.retire(retired)
                for si in retired:
                    dsem_pool.remove(si)
                P.barrier()
            with ExitStack() as stack:
                Yb = [(Buf(P, stack, f"Y1_{i}", [128, D], F32), Buf(P, stack, f"Y2_{i}", [128, D], F32), dsem(), dsem())
                      for i in range(8)]
                lnt = [(Buf(P, stack, f"STe{i}", [128, 2, 6], F32), Buf(P, stack, f"MVe{i}", [128, 2], F32),
                        Buf(P, stack, f"SDe{i}", [128, 4], F32)) for i in range(8)]
                def combine(b):
                    gb = (LNB.t[:, 0, :], LNB.t[:, 1, :])

                    def A(t):
                        Y1, Y2, sy1, sy2 = Yb[t % 8]
                        P.idma(Y1.t[:], YD[:, :], sy1, in_off=SL1I.t[:, t:t + 1], r=[SL1I.r0], w=[Y1.r0])
                        P.idma(Y2.t[:], YD[:, :], sy2, in_off=SL2I.t[:, t:t + 1], r=[SL2I.r0], w=[Y2.r0])
                        P.op("act", lambda h: h.activation(out=Y1.t[:], in_=Y1.t[:], func=AF.Identity, scale=G12.t[:, 0, t:t + 1]),
                             r=[G12.r0], w=[Y1.r0])
                        P.op("dve", lambda h: h.scalar_tensor_tensor(out=Y1.t[:], in0=Y2.t[:], scalar=G12.t[:, 1, t:t + 1], in1=Y1.t[:],
                                                                     op0=ALU.mult, op1=ALU.add),
                             r=[G12.r0, Y2.r0], w=[Y1.r0])
                        P.op("dve", lambda h: h.scalar_tensor_tensor(out=Y1.t[:], in0=X[:, t, :], scalar=alpha, in1=Y1.t[:],
                                                                     op0=ALU.mult, op1=ALU.add),
                             r=[Xr[t]], w=[Y1.r0])
                        ln_stats(lnt[t % 8], Y1.t[:], [Y1.r0])

                    def B(t):
                        Y1 = Yb[t % 8][0]
                        ln_norm(lnt[t % 8], Y1.t[:], [Y1.r0])

                    def C(t):
                        Y1 = Yb[t % 8][0]
                        ln_affine(Y1.t[:], [Y1.r0], gb, LNB.r0, X[:, t, :], Xr[t])
                    t0 = 4 * b
                    A(t0)
                    A(t0 + 1)
                    B(t0)
                    A(t0 + 2)
                    B(t0 + 1)
                    C(t0)
                    A(t0 + 3)
                    B(t0 + 2)
                    C(t0 + 1)
                    B(t0 + 3)
                    C(t0 + 2)
                    C(t0 + 3)
                ple_phase(l, l == L - 1, pre=combine)

    def ple_parts(stack, l, nxt=2):
        PG = Buf(P, stack, "PG", [128, KC, D], BF16)
        PLW = Buf(P, stack, "PLW", [128, 2, D], BF16)
        PTs = [(Buf(P, stack, f"PT{i}", [128, 2, 512], BF16), dsem()) for i in range(nxt)]
        XTs = [Buf(P, stack, f"XTp{i}", [128, KC, 512], BF16, nres=8) for i in range(nxt)]
        SG = [Buf(P, stack, f"SG{i}", [128, 512], F32) for i in range(2)]
        sems = (dsem(), dsem())

        def start_loads():
            load_w("pool", PG, ple_gate_d[l].rearrange("(k p) f -> p k f", p=128), sems[0])
            load_w("pool", PLW, ple_w_d[l].rearrange("(k p) f -> p k f", p=128), sems[1])

        def emit1(b):
            XT = XTs[b % nxt]
            PT, ptsem = PTs[b % nxt]
            P.dma("pool", PT.t[:], pT_d[l][:, b * 512:(b + 1) * 512].rearrange("(k p) s -> p k s", p=128), ptsem, w=[PT.r0],
                  max_dma_last_dim=8192)
            to_fm(XT, b)

        def emit(b, last, after_fm=None):
            emit1(b)
            if after_fm is not None:
                after_fm()
            emit2(b, last)

        def emit2(b, last):
            XT = XTs[b % nxt]
            PT, ptsem = PTs[b % nxt]
            for tt in range(4):
                t = 4 * b + tt
                for hh in range(2):
                    bg, bp = nb(), nb()

                    def fg(h, tt=tt, hh=hh, bg=bg):
                        for kc in range(KC):
                            ins = h.matmul(bg[0][:], lhsT=XT.t[:, kc, tt * 128:(tt + 1) * 128], rhs=PG.t[:, kc, hh * 512:(hh + 1) * 512],
                                           start=(kc == 0), stop=(kc == KC - 1))
                        return ins
                    P.op("pe", fg, r=[PG.r0, XT.r[2 * tt], XT.r[2 * tt + 1]], w=[bg[1]])

                    def fp(h, tt=tt, hh=hh, bp=bp):
                        for kc in range(2):
                            ins = h.matmul(bp[0][:], lhsT=PT.t[:, kc, tt * 128:(tt + 1) * 128], rhs=PLW.t[:, kc, hh * 512:(hh + 1) * 512],
                                           start=(kc == 0), stop=(kc == 1))
                        return ins
                    P.op("pe", fp, r=[PLW.r0, PT.r0], w=[bp[1]])
                    sg = SG[hh]
                    P.op("act", lambda h, bg=bg, sg=sg: h.activation(out=sg.t[:], in_=bg[0][:], func=AF.Sigmoid), r=[bg[1]], w=[sg.r0])
                    P.op("dve", lambda h, bp=bp, sg=sg: h.tensor_tensor(out=sg.t[:], in0=bp[0][:], in1=sg.t[:], op=ALU.mult),
                         r=[bp[1]], w=[sg.r0])
                    xo = X[:, t, hh * 512:(hh + 1) * 512]
                    P.op("dve", lambda h, xo=xo, sg=sg: h.tensor_tensor(out=xo, in0=xo, in1=sg.t[:], op=ALU.add),
                         r=[sg.r0] + XT.r, w=[Xr[t]])
                if last:
                    P.dma("sp", out_d[t * 128:(t + 1) * 128, :], X[:, t, :], s_out, r=[Xr[t]])
        return emit, start_loads, emit1, emit2

    def ple_phase(l, last, pre=None):
        with ExitStack() as stack:
            emit, start_loads, _, _ = ple_parts(stack, l)
            if pre is None:
                start_loads()
            else:
                pre(0)
            for b in range(NB):
                def af(b=b):
                    if b + 1 < NB:
                        pre(b + 1)
                    if b == 0:
                        start_loads()
                emit(b, last, after_fm=af if pre is not None else None)
            P.barrier()

    for l in range(L):
        mix_phase(l)
        if l % 2 == 0:
            dense_phase(l)
        else:
            moe_phase(l)
    nc.sync.wait_ge(P.sems[s_out], P.semval[s_out])
    P.st.close()
    return nc


def prep_shared(cfg, inp):
    L, E, FFD, FFE = cfg.L, cfg.E, cfg.FFD, cfg.FFE
    f32 = np.float32
    sh = {}
    for k in ("w_in", "pool_w", "conv_pw", "w_out", "ple_gate_w", "ple_w", "dense_w2"):
        sh[k] = np.ascontiguousarray(inp[k], dtype=f32)

    def w13(w, FF):
        n = w.shape[0]
        return np.ascontiguousarray(w.reshape(n, KC, 128, FF // 256, 256).transpose(0, 3, 2, 1, 4).reshape(n, FF // 256, 128, KC * 256))
    sh["dense_w1"] = w13(np.asarray(inp["dense_w1"], f32), FFD)
    sh["dense_w3"] = w13(np.asarray(inp["dense_w3"], f32), FFD)
    if L // 2:
        NM = L // 2
        NE = NM * E
        sh["router_w"] = np.ascontiguousarray(inp["router_w"], dtype=f32)
        sh["exp_w1"] = w13(np.asarray(inp["exp_w1"], f32).reshape(NE, D, FFE), FFE).reshape(NE * (FFE // 256) * 128, KC * 256)
        sh["exp_w3"] = w13(np.asarray(inp["exp_w3"], f32).reshape(NE, D, FFE), FFE).reshape(NE * (FFE // 256) * 128, KC * 256)
        w2 = np.asarray(inp["exp_w2"], f32).reshape(NE, FFE // 512, 4, 128, 2, 512)
        sh["exp_w2"] = np.ascontiguousarray(w2.transpose(0, 4, 1, 3, 2, 5)).reshape(NE * 2 * (FFE // 512) * 128, 4 * 512)
    pp = np.zeros((128, L * NPP), f32)
    for l in range(L):
        o = l * NPP
        pp[:, o + 0:o + 4] = np.asarray(inp["pool_scale"][l], f32).reshape(4, 128).T
        pp[:, o + 4:o + 8] = np.asarray(inp["conv_b"][l], f32).reshape(4, 128).T
        pp[:, o + 8:o + 12] = np.asarray(inp["conv_ln_g"][l], f32).reshape(4, 128).T
        pp[:, o + 12:o + 16] = np.asarray(inp["conv_ln_b"][l], f32).reshape(4, 128).T
        cw = np.asarray(inp["conv_w"][l], f32)
        pp[:, o + 16:o + 16 + 4 * CW] = cw.reshape(CW, 4, 128).transpose(2, 1, 0).reshape(128, 4 * CW)
    sh["pp"] = pp
    lnb = np.zeros((L * 4, D), f32)
    for l in range(L):
        lnb[4 * l + 0] = inp["ln1_g"][l]
        lnb[4 * l + 1] = inp["ln1_b"][l]
        lnb[4 * l + 2] = inp["ln2_g"][l]
        lnb[4 * l + 3] = inp["ln2_b"][l]
    sh["lnb"] = lnb
    NSB = (2 * cfg.S + E * 511) // 512
    NG13, NJG = FFE // 256, FFE // 512
    CO = const_offsets(NSB, NG13, NJG)
    consts = np.zeros((128, CO["n"]), f32)
    consts[:, 0:128] = np.eye(128, dtype=f32)
    consts[:, 128:256] = 1.0
    for g, w in enumerate(WINS):
        cnt = np.minimum(np.arange(HALO_P) + 1.0, float(w))
        consts[:, 256 + g * HALO_P:256 + (g + 1) * HALO_P] = (1.0 / cnt).astype(f32)[None, :]
    pidx = np.arange(128)
    consts[:, CO["tri"]:CO["tri"] + 128] = (pidx[:, None] <= pidx[None, :]).astype(f32)
    consts[:, CO["biota"]:CO["biota"] + NSB] = np.arange(NSB, dtype=f32)[None, :]
    consts[:, CO["c13"]:CO["c13"] + NG13] = (np.arange(NG13)[None, :] * 128 + pidx[:, None]).astype(f32)
    consts[:, CO["c2"]:CO["c2"] + 2 * NJG] = (np.arange(2 * NJG)[None, :] * 128 + pidx[:, None]).astype(f32)
    sh["junk"] = np.zeros((128, 1), f32)
    sh["tokid"] = (np.arange(cfg.NT)[None, :] * 128 + pidx[:, None]).astype(np.int32)
    sh["consts"] = consts
    return sh


def kernel(**inputs):
    cfg = Cfg()
    n = 8
    nc = build(cfg)
    sh = prep_shared(cfg, inputs)
    x = np.asarray(inputs["x"], np.float32)
    p = np.asarray(inputs["p"], np.float32)
    in_maps = []
    for c in range(n):
        m = dict(sh)
        m["x"] = np.ascontiguousarray(x[c])
        m["pT"] = np.ascontiguousarray(p[:, c].transpose(0, 2, 1))
        in_maps.append(m)
    res = run_bass_kernel_spmd(nc, in_maps, core_ids=list(range(n)))
    return np.stack([r["out"] for r in res.results], axis=0).astype(np.float32)
```

```python
import numpy as np
from contextlib import ExitStack
import concourse.bass as bass
import concourse.mybir as mybir
from concourse.bass_utils import run_bass_kernel_spmd

F32 = mybir.dt.float32
BF16 = mybir.dt.bfloat16
I32 = mybir.dt.int32
AF = mybir.ActivationFunctionType
ALU = mybir.AluOpType

D = 1024
KC = D // 128
DPOOL = 512
DCONV = 512
NGRP = 4
WINS = (2, 4, 8, 16)
CW = 31
DIN = 1536
DPLE = 256
LN_EPS = 1e-5
HALO_P = 16
HALO_C = 32
NPP = 16 + 4 * CW


def const_offsets(NSB, NG13, NJG):
    o = {}
    n = 256 + NGRP * HALO_P
    for k, w in (("tri", 128), ("biota", NSB), ("c13", NG13), ("c2", 2 * NJG)):
        o[k] = n
        n += w
    o["n"] = n
    return o


class Cfg:
    def __init__(self, S=2048, FFD=2816, E=8, FFE=3584, L=2):
        self.S, self.FFD, self.E, self.FFE, self.L = S, FFD, E, FFE, L
        self.NT = S // 128
        self.NB = S // 512
        self.alpha = (2.0 * L) ** 0.25


class Res:
    __slots__ = ("n", "lw", "rd", "x")

    def __init__(self, n, x=False):
        self.n, self.lw, self.rd, self.x = n, None, {}, x


class Eng:
    def __init__(self, name, h, si):
        self.name, self.h, self.si, self.seen = name, h, si, {}


class Prog:
    def __init__(self):
        self.nc = bass.Bass("TRN2", target_bir_lowering=False)
        self.st = ExitStack()
        self.sems, self.semval, self.allres = [], [], []
        nc = self.nc
        self.eng = {}
        for name, h in (("pe", nc.tensor), ("act", nc.scalar), ("dve", nc.vector),
                        ("pool", nc.gpsimd), ("sp", nc.sync)):
            self.eng[name] = Eng(name, h, self.newsem("e_" + name))
        self.nwaits = 0
        self.sem_owner = {}

    def newsem(self, name):
        h = self.st.enter_context(self.nc.semaphore(name))
        self.sems.append(h)
        self.semval.append(0)
        return len(self.sems) - 1

    def res(self, name, x=False):
        r = Res(name, x)
        self.allres.append(r)
        return r

    def _deps(self, r, w):
        deps = {}

        def need(ev):
            if ev is not None and deps.get(ev[0], 0) < ev[1]:
                deps[ev[0]] = ev[1]
        for x in r:
            need(x.lw)
            if x.x:
                for ev in x.rd.items():
                    need(ev)
        for x in w:
            need(x.lw)
            for ev in x.rd.items():
                need(ev)
        return deps

    def _wait(self, eng, deps):
        for si, val in deps.items():
            if eng.name == "pe" and si == eng.si:
                continue
            if eng.seen.get(si, 0) < val:
                eng.h.wait_ge(self.sems[si], val)
                eng.seen[si] = val
                self.nwaits += 1

    def _record(self, ev, r, w):
        for x in w:
            x.lw, x.rd = ev, {}
        for x in r:
            if x.x:
                x.lw, x.rd = ev, {}
            elif x.rd.get(ev[0], 0) < ev[1]:
                x.rd[ev[0]] = ev[1]

    def op(self, e, fn, r=(), w=()):
        eng = self.eng[e]
        self._wait(eng, self._deps(r, w))
        inst = fn(eng.h)
        inst.then_inc(self.sems[eng.si], 1)
        self.semval[eng.si] += 1
        self._record((eng.si, self.semval[eng.si]), r, w)

    def dma(self, q, out, in_, sem, r=(), w=(), **kw):
        eng = self.eng[q]
        self._wait(eng, self._deps(r, w))
        eng.h.dma_start(out=out, in_=in_, **kw).then_inc(self.sems[sem], 16)
        self.sem_owner[sem] = q
        self.semval[sem] += 16
        self._record((sem, self.semval[sem]), r, w)

    def idma(self, out, in_, sem, out_off=None, in_off=None, r=(), w=()):
        eng = self.eng["pool"]
        self._wait(eng, self._deps(r, w))
        oo = bass.IndirectOffsetOnAxis(ap=out_off, axis=0) if out_off is not None else None
        io = bass.IndirectOffsetOnAxis(ap=in_off, axis=0) if in_off is not None else None
        eng.h.indirect_dma_start(out=out, out_offset=oo, in_=in_, in_offset=io).then_inc(self.sems[sem], 16)
        self.sem_owner[sem] = "pool"
        self.semval[sem] += 16
        self._record((sem, self.semval[sem]), r, w)

    def retire(self, sems):
        sems = set(sems)
        for x in self.allres:
            if x.lw is not None and x.lw[0] in sems:
                x.lw = None
            for si in [k for k in x.rd if k in sems]:
                del x.rd[si]
        for e in self.eng.values():
            for si in [k for k in e.seen if k in sems]:
                del e.seen[si]

    def cond(self, cond_expr, body, junk_out, junk_in, fix=None, retired=frozenset()):
        pre = list(self.semval)
        seen0 = {n: dict(e.seen) for n, e in self.eng.items()}
        with self.nc.If(cond_expr):
            body()
        post = list(self.semval)
        with self.nc.Else():
            for name, eng in self.eng.items():
                for si, owner in self.sem_owner.items():
                    if owner == name and post[si] != pre[si] and si not in retired:
                        eng.h.wait_ge(self.sems[si], pre[si])
                        if fix is not None and fix(eng, si, post[si] - pre[si]):
                            continue
                        eng.h.dma_start(out=junk_out[:, si:si + 1], in_=junk_in).then_inc(self.sems[si], post[si] - pre[si])
                d = post[eng.si] - pre[eng.si]
                if d:
                    eng.h.drain().then_inc(self.sems[eng.si], d)
        for n, e in self.eng.items():
            e.seen = seen0[n]

    def barrier(self):
        deps = {}
        for x in self.allres:
            if x.lw is not None and deps.get(x.lw[0], 0) < x.lw[1]:
                deps[x.lw[0]] = x.lw[1]
            for si, v in x.rd.items():
                if deps.get(si, 0) < v:
                    deps[si] = v
        for eng in self.eng.values():
            for si, val in deps.items():
                if eng.seen.get(si, 0) < val:
                    eng.h.wait_ge(self.sems[si], val)
                    eng.seen[si] = val
        self.allres = [x for x in self.allres if getattr(x, "keep", False) or True]


class Alias:
    def __init__(self, t, r):
        self.t, self.r = t, list(r)

    @property
    def r0(self):
        return self.r[0]


class Buf:
    _n = [0]

    def __init__(self, P, stack, name, shape, dt, nres=1):
        Buf._n[0] += 1
        name = f"{name}_{Buf._n[0]}"
        self.t = stack.enter_context(P.nc.sbuf_tensor(name, shape, dt))
        self.r = [P.res(f"{name}.{i}") for i in range(nres)]

    @property
    def r0(self):
        return self.r[0]


def build(cfg):
    P = Prog()
    nc = P.nc
    S, NT, NB, L, E = cfg.S, cfg.NT, cfg.NB, cfg.L, cfg.E
    alpha = cfg.alpha
    FFE_ = cfg.FFE

    def din(name, shape, dt=F32):
        return nc.dram_tensor(name, list(shape), dt, kind="ExternalInput").ap()

    x_d = din("x", [S, D])
    pT_d = din("pT", [L, DPLE, S])
    w_in_d = din("w_in", [L, D, DIN])
    pool_w_d = din("pool_w", [L, NGRP, 128, 128])
    conv_pw_d = din("conv_pw", [L, DCONV, DCONV])
    w_out_d = din("w_out", [L, D, D])
    ple_gate_d = din("ple_gate_w", [L, D, D])
    ple_w_d = din("ple_w", [L, DPLE, D])
    pp_d = din("pp", [128, L * NPP])
    lnb_d = din("lnb", [L * 4, D])
    NSB = (2 * S + E * 511) // 512
    NG13, NJG = FFE_ // 256, FFE_ // 512
    CO = const_offsets(NSB, NG13, NJG)
    consts_d = din("consts", [128, CO["n"]])
    tokid_d = din("tokid", [128, NT], I32)
    junk_d = din("junk", [128, 1])
    ND = (L + 1) // 2
    NM = L // 2
    FFD, FFE = cfg.FFD, cfg.FFE
    dw1_d = din("dense_w1", [ND, FFD // 256, 128, KC * 256])
    dw3_d = din("dense_w3", [ND, FFD // 256, 128, KC * 256])
    dw2_d = din("dense_w2", [ND, FFD, D])
    if NM:
        router_d = din("router_w", [NM, D, E])
        ew1_d = din("exp_w1", [NM * E * (FFE // 256) * 128, KC * 256])
        ew3_d = din("exp_w3", [NM * E * (FFE // 256) * 128, KC * 256])
        ew2_d = din("exp_w2", [NM * E * 2 * (FFE // 512) * 128, 4 * 512])
        X1D = nc.dram_tensor("x1d_scratch", [S, D], F32, kind="Internal").ap()
        SLOTMAP = nc.dram_tensor("slotmap_scratch", [NSB * 512, 1], I32, kind="Internal").ap()
        YD = nc.dram_tensor("yd_scratch", [NSB * 512, D], F32, kind="Internal").ap()
    out_d = nc.dram_tensor("out", [S, D], F32, kind="ExternalOutput").ap()

    st = P.st
    X = st.enter_context(nc.sbuf_tensor("X", [128, NT, D], F32))
    Xr = [P.res(f"X{t}") for t in range(NT)]
    CONST = Buf(P, st, "CONST", [128, CO["n"]], F32)
    PPB = Buf(P, st, "PP", [128, L * NPP], F32)
    ident = CONST.t[:, 0:128]
    ones = CONST.t[:, 128:256]
    cinv = CONST.t[:, 256:256 + NGRP * HALO_P].rearrange("p (g c) -> p g c", g=NGRP)
    tri = CONST.t[:, CO["tri"]:CO["tri"] + 128]
    biota = CONST.t[:, CO["biota"]:CO["biota"] + NSB]
    c13 = CONST.t[:, CO["c13"]:CO["c13"] + NG13]
    c2 = CONST.t[:, CO["c2"]:CO["c2"] + 2 * NJG]
    banks = []
    for i in range(8):
        t = st.enter_context(nc.psum_tensor(f"bank{i}", [128, 512], F32))
        banks.append((t, P.res(f"bank{i}", x=True)))
    bank_rr = [0]

    def nb():
        b = banks[bank_rr[0] % 8]
        bank_rr[0] += 1
        return b

    s_init = P.newsem("init")
    s_out = P.newsem("out")
    s_x = [P.newsem(f"ldx{b}") for b in range(NB)]
    dsem_pool = [P.newsem(f"d{i}") for i in range(38)]
    dsem_next = [0]
    hsem_pool = [P.newsem(f"h{i}") for i in range(14)]
    hsem_next = [0]

    def dsem():
        s = dsem_pool[dsem_next[0] % len(dsem_pool)]
        dsem_next[0] += 1
        return s

    def hsem():
        s = hsem_pool[hsem_next[0] % len(hsem_pool)]
        hsem_next[0] += 1
        return s

    P.dma("sp", CONST.t[:], consts_d[:, :], s_init, w=[CONST.r0])
    P.dma("sp", PPB.t[:], pp_d[:, :], s_init, w=[PPB.r0])
    CONST.r0.lw = PPB.r0.lw = (s_init, P.semval[s_init])
    for b in range(NB):
        P.dma("sp", X[:, 4 * b:4 * b + 4, :], x_d[b * 512:(b + 1) * 512, :].rearrange("(t p) d -> p t d", p=128),
              s_x[b], w=Xr[4 * b:4 * b + 4])

    act_copy_toggle = [0]

    def evac(out, in_, r, w, eng=None):
        if eng is None:
            eng = ("act", "dve")[act_copy_toggle[0] % 2]
            act_copy_toggle[0] += 1
        if eng == "act":
            P.op("act", lambda h: h.copy(out=out, in_=in_), r=r, w=w)
        else:
            P.op("dve", lambda h: h.tensor_copy(out=out, in_=in_), r=r, w=w)

    def to_fm(XT, b, lo=None, src=None):
        for tt in range(4):
            t = 4 * b + tt
            bk = [nb(), nb()]
            sap, sres = (X[:, t, :], Xr[t]) if src is None else src(tt)

            def tr(h, sap=sap, bk=bk):
                for kc in range(KC):
                    ins = h.transpose(out=bk[kc // 4][0][:, (kc % 4) * 128:(kc % 4 + 1) * 128],
                                      in_=sap[:, kc * 128:(kc + 1) * 128], identity=ident)
                return ins
            P.op("pe", tr, r=[sres, CONST.r0], w=[bk[0][1], bk[1][1]])
            for hh in range(2):
                o = XT.t[:, 4 * hh:4 * hh + 4, tt * 128:(tt + 1) * 128]
                i = bk[hh][0][:].rearrange("p (k t) -> p k t", k=4)
                pr = XT.r[tt * 2 + hh]
                if lo is None:
                    evac(o, i, r=[bk[hh][1]], w=[pr])
                else:
                    evac(o, i, r=[bk[hh][1]], w=[pr], eng="act")
                    lo_o = lo.t[:, tt, 4 * hh:4 * hh + 4, :]
                    P.op("dve", lambda h, lo_o=lo_o, i=i, o=o: h.tensor_tensor(out=lo_o, in0=i, in1=o, op=ALU.subtract),
                         r=[bk[hh][1], pr], w=[lo.r[tt * 2 + hh]])

    def ln_stats(ph_tmp, Z, Zr):
        ST, MV, SD = ph_tmp
        P.op("dve", lambda h: h.bn_stats(out=ST.t[:, 0, :], in_=Z[:, 0:512]), r=Zr, w=[ST.r0])
        P.op("dve", lambda h: h.bn_stats(out=ST.t[:, 1, :], in_=Z[:, 512:1024]), r=Zr, w=[ST.r0])
        P.op("dve", lambda h: h.bn_aggr(out=MV.t[:], in_=ST.t[:].rearrange("p a b -> p (a b)")), r=[ST.r0], w=[MV.r0])
        P.op("act", lambda h: h.activation(out=SD.t[:, 0:1], in_=MV.t[:, 1:2], func=AF.Sqrt, bias=epsb, scale=1.0),
             r=[MV.r0, EPS.r0], w=[SD.r0])

    def ln_norm(ph_tmp, Z, Zr):
        ST, MV, SD = ph_tmp
        P.op("dve", lambda h: h.reciprocal(out=SD.t[:, 1:2], in_=SD.t[:, 0:1]), r=[SD.r0], w=[SD.r0])
        P.op("dve", lambda h: h.tensor_scalar(out=SD.t[:, 2:3], in0=MV.t[:, 0:1], scalar1=SD.t[:, 1:2], scalar2=-1.0,
                                              op0=ALU.mult, op1=ALU.mult), r=[MV.r0], w=[SD.r0])
        P.op("act", lambda h: h.activation(out=Z, in_=Z, func=AF.Identity, scale=SD.t[:, 1:2], bias=SD.t[:, 2:3]),
             r=[SD.r0], w=Zr)

    def ln_affine(Z, Zr, gb, gbr, out_ap, out_r, extra_r=()):
        P.op("dve", lambda h: h.tensor_tensor(out=Z, in0=Z, in1=gb[0], op=ALU.mult), r=[gbr], w=Zr)
        P.op("dve", lambda h: h.tensor_tensor(out=out_ap, in0=Z, in1=gb[1], op=ALU.add), r=Zr + [gbr] + list(extra_r), w=[out_r])

    def layer_norm(ph_tmp, Z, Zr, gb, gbr, out_ap, out_r, extra_r=()):
        Zr = list(Zr) if isinstance(Zr, (list, tuple)) else [Zr]
        ln_stats(ph_tmp, Z, Zr)
        ln_norm(ph_tmp, Z, Zr)
        ln_affine(Z, Zr, gb, gbr, out_ap, out_r, extra_r)

    EPS = Buf(P, st, "EPS", [128, 1], F32)
    P.op("dve", lambda h: h.memset(EPS.t[:], LN_EPS), w=[EPS.r0])
    epsb = EPS.t[:, 0:1]

    def load_w(q, buf, src, sem, **kw):
        P.dma(q, buf.t[:], src, sem, w=[buf.r0], max_dma_last_dim=8192, **kw)

    def mix_phase(l):
        with ExitStack() as ph:
            WIN = Buf(P, ph, "WIN", [128, KC, DIN], BF16, nres=3)
            WOUT = Buf(P, ph, "WOUT", [128, KC, D], BF16)
            CPW = Buf(P, ph, "CPW", [128, 4, DCONV], BF16)
            PW = Buf(P, ph, "PW", [128, NGRP, 128], BF16)
            LNB = Buf(P, ph, "LNB1", [128, 2, D], F32)
            XT = Buf(P, ph, "XTm", [128, KC, 512], BF16, nres=8)
            U = Buf(P, ph, "U", [128, NGRP, HALO_P + 512], F32, nres=NGRP + 1)
            PA = Buf(P, ph, "PA", [128, HALO_P + 512], F32)
            PB = Buf(P, ph, "PB", [128, HALO_P + 512], F32)
            DP = Buf(P, ph, "DPL", [128, NGRP, 512], BF16, nres=NGRP)
            V = Buf(P, ph, "V", [128, 4, HALO_C + 512], BF16, nres=5)
            DG = [Buf(P, ph, f"DG{i}", [128, CW, 128], BF16) for i in range(4)]
            YCs = [Buf(P, ph, "YC", [128, 4, 512], F32, nres=4)] * 2
            SN = Buf(P, ph, "SN", [128, 4, 512], BF16, nres=4)
            MIP = [Buf(P, ph, "MIP", [128, 4, 512], BF16, nres=4)] * 2
            MIC = Buf(P, ph, "MIC", [128, 4, 512], BF16, nres=4)
            TMP = [Buf(P, ph, f"TMPm{i}", [128, 512], F32) for i in range(2)]
            SQ = TMP
            MEAN = Alias(PA.t[:, 0:512], PA.r)
            VAR = Alias(PB.t[:, 0:512], PB.r)
            RSTD = Buf(P, ph, "RSTD", [128, 512], F32)
            T16 = Buf(P, ph, "T16", [128, HALO_P], F32)
            lnt = [(Buf(P, ph, f"STm{i}", [128, 2, 6], F32), Buf(P, ph, f"MVm{i}", [128, 2], F32),
                    Buf(P, ph, f"SDm{i}", [128, 4], F32)) for i in range(2)]

            for i in range(3):
                P.dma("pool", WIN.t[:, :, i * 512:(i + 1) * 512], w_in_d[l][:, i * 512:(i + 1) * 512].rearrange("(k p) f -> p k f", p=128),
                      dsem(), w=[WIN.r[i]], max_dma_last_dim=8192)
            load_w("pool", PW, pool_w_d[l].rearrange("g c d -> c g d"), dsem())
            load_w("pool", CPW, conv_pw_d[l].rearrange("(k p) f -> p k f", p=128), dsem())
            load_w("pool", WOUT, w_out_d[l].rearrange("(k p) f -> p k f", p=128), dsem())
            P.dma("sp", LNB.t[:], lnb_d[4 * l:4 * l + 2, :].partition_broadcast(128), hsem(), w=[LNB.r0])
            pp0 = l * NPP
            P.op("dve", lambda h: h.memset(U.t[:, :, 0:HALO_P], 0.0), w=U.r)
            P.op("dve", lambda h: h.memset(V.t[:, :, 0:HALO_C], 0.0), w=V.r)
            W = HALO_P + 512
            for c in range(4):
                def mk(h, c=c):
                    for k in range(CW):
                        col = pp0 + 16 + c * CW + k
                        ins = h.tensor_scalar(out=DG[c].t[:, k, :], in0=ident, scalar1=PPB.t[:, col:col + 1], scalar2=None,
                                              op0=ALU.mult)
                    return ins
                P.op("dve", mk, r=[CONST.r0, PPB.r0], w=[DG[c].r0])

            def inproj(oc, bk):
                def f(h):
                    for kc in range(KC):
                        ins = h.matmul(bk[0][:], lhsT=WIN.t[:, kc, oc * 128:(oc + 1) * 128], rhs=XT.t[:, kc, :],
                                       start=(kc == 0), stop=(kc == KC - 1))
                    return ins
                P.op("pe", f, r=[WIN.r[oc // 4]] + XT.r, w=[bk[1]])

            def S0(b):
                to_fm(XT, b)

            def S1(b):
                for g in range(NGRP):
                    bk = nb()
                    inproj(g, bk)
                    evac(U.t[:, g, HALO_P:], bk[0][:], r=[bk[1]], w=[U.r[g]])
                for c in range(4):
                    bv, bg = nb(), nb()
                    inproj(4 + c, bv)
                    inproj(8 + c, bg)
                    tmp = TMP[c % 2]
                    P.op("act", lambda h, bg=bg, tmp=tmp: h.activation(out=tmp.t[:], in_=bg[0][:], func=AF.Sigmoid),
                         r=[bg[1]], w=[tmp.r0])
                    P.op("dve", lambda h, bv=bv, tmp=tmp, c=c: h.tensor_tensor(out=V.t[:, c, HALO_C:], in0=bv[0][:], in1=tmp.t[:],
                                                                                op=ALU.mult),
                         r=[bv[1], tmp.r0], w=[V.r[c]])

            def S2p(b):
                MI = MIP[b % 2]
                for g in range(NGRP):
                    src, srcr = U.t[:, g, :], U.r[g]
                    pp = [PA, PB]
                    k, lo = 1, 0
                    for step in range(g + 1):
                        dst = pp[step % 2]
                        P.op("dve", lambda h, dst=dst, src=src, k=k, lo=lo: h.tensor_tensor(
                            out=dst.t[:, lo + k:W], in0=src[:, lo + k:W], in1=src[:, lo:W - k], op=ALU.add),
                             r=[srcr], w=[dst.r0])
                        src, srcr = dst.t[:], dst.r0
                        lo += k
                        k *= 2
                    w = WINS[g]
                    P.op("dve", lambda h, src=src, g=g, w=w: h.scalar_tensor_tensor(out=DP.t[:, g, :], in0=src[:, HALO_P:W],
                                                                                    scalar=1.0 / w, in1=U.t[:, g, HALO_P:W],
                                                                                    op0=ALU.mult, op1=ALU.subtract),
                         r=[srcr, U.r[g]], w=[DP.r[g]])
                    if b == 0:
                        P.op("dve", lambda h, src=src, g=g: h.tensor_tensor(out=T16.t[:], in0=src[:, HALO_P:2 * HALO_P],
                                                                            in1=cinv[:, g, :], op=ALU.mult),
                             r=[srcr, CONST.r0], w=[T16.r0])
                        P.op("dve", lambda h, g=g: h.tensor_tensor(out=DP.t[:, g, 0:HALO_P], in0=T16.t[:],
                                                                   in1=U.t[:, g, HALO_P:2 * HALO_P], op=ALU.subtract),
                             r=[T16.r0, U.r[g]], w=[DP.r[g]])
                    bk = nb()
                    P.op("pe", lambda h, g=g, bk=bk: h.matmul(bk[0][:], lhsT=PW.t[:, g, :], rhs=DP.t[:, g, :], start=True, stop=True),
                         r=[PW.r0, DP.r[g]], w=[bk[1]])
                    P.op("act", lambda h, g=g, bk=bk: h.activation(out=MI.t[:, g, :], in_=bk[0][:], func=AF.Identity,
                                                                   scale=PPB.t[:, pp0 + g:pp0 + g + 1]),
                         r=[bk[1], PPB.r0], w=[MI.r[g]])
                if b + 1 < NB:
                    P.op("dve", lambda h: h.tensor_copy(out=U.t[:, :, 0:HALO_P], in_=U.t[:, :, 512:512 + HALO_P]),
                         r=[], w=U.r)

            def S2c(b):
                YC = YCs[b % 2]
                for c in range(4):
                    dg = DG[c]
                    bk = nb()

                    def cv(h, c=c, dg=dg, bk=bk):
                        for k in range(CW):
                            ins = h.matmul(bk[0][:], lhsT=dg.t[:, k, :], rhs=V.t[:, c, 2 + k:2 + k + 512],
                                           start=(k == 0), stop=(k == CW - 1))
                        return ins
                    P.op("pe", cv, r=[dg.r0, V.r[c]], w=[bk[1]])
                    P.op("act", lambda h, c=c, bk=bk: h.activation(out=YC.t[:, c, :], in_=bk[0][:], func=AF.Identity,
                                                                   bias=PPB.t[:, pp0 + 4 + c:pp0 + 5 + c], scale=1.0),
                         r=[bk[1], PPB.r0], w=[YC.r[c]])
                if b + 1 < NB:
                    P.op("dve", lambda h: h.tensor_copy(out=V.t[:, :, 0:HALO_C], in_=V.t[:, :, 512:512 + HALO_C]),
                         r=[], w=V.r)

            def S3(b):
                YC = YCs[b % 2]
                b1, b2 = nb(), nb()

                def s1(h, b1=b1):
                    for c in range(4):
                        ins = h.matmul(b1[0][:], lhsT=ones, rhs=YC.t[:, c, :], start=(c == 0), stop=(c == 3))
                    return ins
                P.op("pe", s1, r=[CONST.r0] + YC.r, w=[b1[1]])
                for c in range(4):
                    sq = SQ[c % 2]
                    P.op("dve", lambda h, c=c, sq=sq: h.tensor_tensor(out=sq.t[:], in0=YC.t[:, c, :], in1=YC.t[:, c, :], op=ALU.mult),
                         r=[YC.r[c]], w=[sq.r0])
                    P.op("pe", lambda h, c=c, sq=sq, b2=b2: h.matmul(b2[0][:], lhsT=ones, rhs=sq.t[:], start=(c == 0), stop=(c == 3)),
                         r=[CONST.r0, sq.r0], w=[b2[1]])
                P.op("act", lambda h, b1=b1: h.mul(out=MEAN.t[:], in_=b1[0][:], mul=1.0 / DCONV), r=[b1[1]], w=[MEAN.r0])
                P.op("dve", lambda h: h.tensor_tensor(out=VAR.t[:], in0=MEAN.t[:], in1=MEAN.t[:], op=ALU.mult),
                     r=[MEAN.r0], w=[VAR.r0])
                P.op("dve", lambda h, b2=b2: h.scalar_tensor_tensor(out=VAR.t[:], in0=b2[0][:], scalar=1.0 / DCONV, in1=VAR.t[:],
                                                                   op0=ALU.mult, op1=ALU.subtract),
                     r=[b2[1]], w=[VAR.r0])
                P.op("act", lambda h: h.activation(out=VAR.t[:], in_=VAR.t[:], func=AF.Sqrt, bias=epsb, scale=1.0),
                     r=[EPS.r0], w=[VAR.r0])
                P.op("dve", lambda h: h.reciprocal(out=RSTD.t[:], in_=VAR.t[:]), r=[VAR.r0], w=[RSTD.r0])

            def S4(b):
                YC = YCs[b % 2]
                for c in range(4):
                    tmp = TMP[c % 2]
                    P.op("dve", lambda h, c=c, tmp=tmp: h.tensor_tensor(out=tmp.t[:], in0=YC.t[:, c, :], in1=MEAN.t[:], op=ALU.subtract),
                         r=[YC.r[c], MEAN.r0], w=[tmp.r0])
                    P.op("dve", lambda h, tmp=tmp: h.tensor_tensor(out=tmp.t[:], in0=tmp.t[:], in1=RSTD.t[:], op=ALU.mult),
                         r=[RSTD.r0], w=[tmp.r0])
                    P.op("act", lambda h, c=c, tmp=tmp: h.activation(out=SN.t[:, c, :], in_=tmp.t[:], func=AF.Silu,
                                                                     scale=PPB.t[:, pp0 + 8 + c:pp0 + 9 + c],
                                                                     bias=PPB.t[:, pp0 + 12 + c:pp0 + 13 + c]),
                         r=[tmp.r0, PPB.r0], w=[SN.r[c]])

            def S4p(b):
                for oc in range(4):
                    bk = nb()

                    def pw(h, oc=oc, bk=bk):
                        for kc in range(4):
                            ins = h.matmul(bk[0][:], lhsT=CPW.t[:, kc, oc * 128:(oc + 1) * 128], rhs=SN.t[:, kc, :],
                                           start=(kc == 0), stop=(kc == 3))
                        return ins
                    P.op("pe", pw, r=[CPW.r0] + SN.r, w=[bk[1]])
                    evac(MIC.t[:, oc, :], bk[0][:], r=[bk[1]], w=[MIC.r[oc]])

            def S5(b):
                MI = MIP[b % 2]
                YC = YCs[b % 2]
                Z = [Alias(YC.t[:, 2 * i:2 * i + 2, :].rearrange("p a b -> p (a b)"), YC.r[2 * i:2 * i + 2]) for i in range(2)]
                gb = (LNB.t[:, 0, :], LNB.t[:, 1, :])

                def A(tt):
                    t = 4 * b + tt
                    z = Z[tt % 2]
                    for hh in range(2):
                        bk = nb()

                        def wo(h, tt=tt, hh=hh, bk=bk):
                            for kc in range(KC):
                                src = MI.t[:, kc, tt * 128:(tt + 1) * 128] if kc < 4 else MIC.t[:, kc - 4, tt * 128:(tt + 1) * 128]
                                ins = h.matmul(bk[0][:], lhsT=src, rhs=WOUT.t[:, kc, hh * 512:(hh + 1) * 512],
                                               start=(kc == 0), stop=(kc == KC - 1))
                            return ins
                        P.op("pe", wo, r=[WOUT.r0] + MI.r + MIC.r, w=[bk[1]])
                        P.op("dve", lambda h, t=t, hh=hh, bk=bk, z=z: h.scalar_tensor_tensor(
                            out=z.t[:, hh * 512:(hh + 1) * 512], in0=X[:, t, hh * 512:(hh + 1) * 512], scalar=alpha,
                            in1=bk[0][:], op0=ALU.mult, op1=ALU.add), r=[bk[1], Xr[t]], w=z.r)
                    ln_stats(lnt[tt % 2], z.t[:], list(z.r))

                def B(tt):
                    z = Z[tt % 2]
                    ln_norm(lnt[tt % 2], z.t[:], list(z.r))

                def C(tt):
                    t = 4 * b + tt
                    z = Z[tt % 2]
                    ln_affine(z.t[:], list(z.r), gb, LNB.r0, X[:, t, :], Xr[t])
                for step in (A, 0), (A, 1), (B, 0), (B, 1), (C, 0), (A, 2), (C, 1), (A, 3), (B, 2), (B, 3), (C, 2), (C, 3):
                    step[0](step[1])

            S0(0)
            S1(0)
            S2c(0)
            S2p(0)
            for b in range(NB):
                if b + 1 < NB:
                    S0(b + 1)
                S3(b)
                S4(b)
                if b + 1 < NB:
                    S1(b + 1)
                S4p(b)
                S5(b)
                if b + 1 < NB:
                    S2c(b + 1)
                    S2p(b + 1)
            P.barrier()

    def ffn_block(ph, XT, ld13, ld2, FF, rings, G, consume, mid=None, mid2=None):
        W1S, W3S, W2S = rings
        NJ = FF // 128
        for g in range(FF // 256):
            s1 = W1S[g % len(W1S)]
            s3 = W3S[g % len(W3S)]
            ld13(0, g, s1)
            ld13(1, g, s3)
            for jj in range(2):
                j = 2 * g + jj
                bk1, bk3 = nb(), nb()
                for (ws, bk) in ((s1, bk1), (s3, bk3)):
                    def f(h, ws=ws, bk=bk, jj=jj):
                        for kc in range(KC):
                            ins = h.matmul(bk[0][:], lhsT=ws[0].t[:, kc, jj * 128:(jj + 1) * 128], rhs=XT.t[:, kc, :],
                                           start=(kc == 0), stop=(kc == KC - 1))
                        return ins
                    P.op("pe", f, r=[ws[0].r0] + XT.r, w=[bk[1]])
                tmp = ph["SIL"][j % 2]
                P.op("act", lambda h, bk1=bk1, tmp=tmp: h.activation(out=tmp.t[:], in_=bk1[0][:], func=AF.Silu),
                     r=[bk1[1]], w=[tmp.r0])
                P.op("dve", lambda h, bk3=bk3, tmp=tmp, j=j: h.tensor_tensor(out=G.t[:, j, :], in0=bk3[0][:], in1=tmp.t[:], op=ALU.mult),
                     r=[bk3[1], tmp.r0], w=[G.r[j]])
        if mid is not None:
            mid()
        bounds = list(range(0, NJ, 4)) + [NJ]
        ngr = len(bounds) - 1
        for hh in range(2):
            yb = [nb() for _ in range(4)]
            for gi in range(ngr):
                j0, j1 = bounds[gi], bounds[gi + 1]
                ws = W2S[(hh * ngr + gi) % len(W2S)]
                ld2(hh, gi, j0, j1, ws)
                for tt in range(4):
                    def f(h, tt=tt, j0=j0, j1=j1, ws=ws):
                        for j in range(j0, j1):
                            ins = h.matmul(yb[tt][0][:], lhsT=G.t[:, j, tt * 128:(tt + 1) * 128], rhs=ws[0].t[:, j - j0, :],
                                           start=(j == 0), stop=(j == NJ - 1))
                        return ins
                    P.op("pe", f, r=[ws[0].r0] + G.r[j0:j1], w=[yb[tt][1]])
            for tt in range(4):
                consume(tt, hh, yb[tt])
            if hh == 0 and mid2 is not None:
                mid2()

    def plain_loaders(w1, w3, w2nat):
        def ld13(which, g, slot):
            src = (w1, w3)[which][g].rearrange("p (k f) -> p k f", k=KC)
            P.dma("pool", slot[0].t[:], src, slot[1], w=[slot[0].r0], max_dma_last_dim=8192)

        def ld2(hh, gi, j0, j1, slot):
            src = w2nat[j0 * 128:j1 * 128, hh * 512:(hh + 1) * 512].rearrange("(j p) d -> p j d", p=128)
            P.dma("pool", slot[0].t[:, 0:j1 - j0, :], src, slot[1], w=[slot[0].r0], max_dma_last_dim=8192)
        return ld13, ld2

    def make_rings(ph, stack):
        W1S = [(Buf(P, stack, f"W1S{i}", [128, KC, 256], BF16), dsem()) for i in range(3)]
        W3S = [(Buf(P, stack, f"W3S{i}", [128, KC, 256], BF16), dsem()) for i in range(3)]
        W2S = [(Buf(P, stack, f"W2S{i}", [128, 4, 512], BF16), dsem()) for i in range(4)]
        ph["SIL"] = [Buf(P, stack, f"SIL{i}", [128, 512], F32) for i in range(2)]
        return (W1S, W3S, W2S)

    def dense_phase(l):
        j = l // 2
        with ExitStack() as stack:
            ph = {}
            rings = make_rings(ph, stack)
            NJ = FFD // 128
            G = Buf(P, stack, "Gd", [128, NJ, 512], BF16, nres=NJ)
            XTs = [Buf(P, stack, f"XTf{i}", [128, KC, 512], BF16, nres=8) for i in range(2)]
            LNB = Buf(P, stack, "LNB2", [128, 2, D], F32)
            Z = Buf(P, stack, "Zf", [128, 4, D], F32, nres=4)
            lnt = [(Buf(P, stack, f"STf{i}", [128, 2, 6], F32), Buf(P, stack, f"MVf{i}", [128, 2], F32),
                    Buf(P, stack, f"SDf{i}", [128, 4], F32)) for i in range(4)]
            P.dma("sp", LNB.t[:], lnb_d[4 * l + 2:4 * l + 4, :].partition_broadcast(128), hsem(), w=[LNB.r0])
            ple_emit, ple_loads, ple1, ple2 = ple_parts(stack, l, nxt=1)
            last_layer = (l == L - 1)
            to_fm(XTs[0], 0)
            for b in range(NB):
                XT = XTs[b % 2]

                def mid(b=b):
                    if b + 1 < NB:
                        to_fm(XTs[(b + 1) % 2], b + 1)
                    if b == 0:
                        ple_loads()
                    if b >= 1:
                        ple1(b - 1)

                def mid2(b=b):
                    if b >= 1:
                        ple2(b - 1, last_layer)

                def consume(tt, hh, bk, b=b):
                    t = 4 * b + tt
                    P.op("dve", lambda h: h.scalar_tensor_tensor(
                        out=Z.t[:, tt, hh * 512:(hh + 1) * 512], in0=X[:, t, hh * 512:(hh + 1) * 512], scalar=alpha,
                        in1=bk[0][:], op0=ALU.mult, op1=ALU.add), r=[bk[1], Xr[t]], w=[Z.r[tt]])
                    if hh == 1:
                        gb = (LNB.t[:, 0, :], LNB.t[:, 1, :])
                        zt = lambda i: Z.t[:, i, :]
                        aff = lambda i: ln_affine(zt(i), [Z.r[i]], gb, LNB.r0, X[:, 4 * b + i, :], Xr[4 * b + i])
                        ln_stats(lnt[tt], zt(tt), [Z.r[tt]])
                        if tt >= 1:
                            ln_norm(lnt[tt - 1], zt(tt - 1), [Z.r[tt - 1]])
                        if tt >= 2:
                            aff(tt - 2)
                        if tt == 3:
                            ln_norm(lnt[3], zt(3), [Z.r[3]])
                            aff(2)
                            aff(3)
                ld13, ld2 = plain_loaders(dw1_d[j], dw3_d[j], dw2_d[j])
                ffn_block(ph, XT, ld13, ld2, FFD, rings, G, consume, mid=mid, mid2=mid2)
            ple_emit(NB - 1, last_layer)
            P.barrier()

    def moe_phase(l):
        jm = l // 2
        NJ = FFE // 128
        NC4 = NSB * 4
        AX = mybir.AxisListType.X
        with ExitStack() as outer:
            LG = Buf(P, outer, "LG", [128, NT, E], F32, nres=NT)
            M8 = Buf(P, outer, "M8", [128, NT, 8], F32, nres=NT)
            MK = Buf(P, outer, "MK", [128, NT, E], F32, nres=NT)
            OH1 = Buf(P, outer, "OH1", [128, NT, E], F32, nres=NT)
            GT = Buf(P, outer, "GT", [128, NT, E], F32, nres=NT)
            SM = Buf(P, outer, "SM", [128, NT, 4], F32, nres=NT)
            SL1I = Buf(P, outer, "SL1I", [128, NT], I32)
            SL2I = Buf(P, outer, "SL2I", [128, NT], I32)
            G12 = Buf(P, outer, "G12", [128, 2, NT], F32)
            GIDX = Buf(P, outer, "GIDX", [128, NC4], I32)
            IDX13 = Buf(P, outer, "IDX13", [128, NSB, NG13], I32)
            IDX2 = Buf(P, outer, "IDX2", [128, NSB, 2 * NJG], I32)
            TOKID = Buf(P, outer, "TOKID", [128, NT], I32)
            NBTI = Buf(P, outer, "NBTI", [128, 1], I32)
            JUNK = Buf(P, outer, "JUNK", [128, 64], F32)
            ZT = Buf(P, outer, "ZT", [128, D], F32)
            P.op("dve", lambda h: h.memset(ZT.t[:], 0.0), w=[ZT.r0])
            LNB = Buf(P, outer, "LNB2e", [128, 2, D], F32)
            X1Dr = [P.res(f"x1d{t}") for t in range(NT)]
            SMr = P.res("slotmap")
            YDr = [P.res(f"yd{b}") for b in range(NSB)]
            s_x1d, s_sm = hsem(), hsem()
            s_yd = [hsem() for _ in range(4)]
            P.dma("sp", LNB.t[:], lnb_d[4 * l + 2:4 * l + 4, :].partition_broadcast(128), hsem(), w=[LNB.r0])
            P.dma("sp", TOKID.t[:], tokid_d[:, :], hsem(), w=[TOKID.r0])
            with ExitStack() as stack:
                XT = Buf(P, stack, "XTr", [128, KC, 512], BF16, nres=8)
                LO = Buf(P, stack, "XLO", [128, 4, KC, 128], BF16, nres=8)
                RW = Buf(P, stack, "RW", [128, KC, E], F32)
                RWH = Buf(P, stack, "RWH", [128, KC, E], BF16)
                RWL = Buf(P, stack, "RWL", [128, KC, E], BF16)
                CS = Buf(P, stack, "CS", [128, NT, E], F32)
                WS = Buf(P, stack, "WS", [128, NT, E], F32)
                OFF = Buf(P, stack, "OFF", [128, NT, E], F32)
                SLOT = Buf(P, stack, "SLOT", [128, NT, E], F32)
                PR = Buf(P, stack, "PR", [128, NT, E], F32)
                OH2 = Buf(P, stack, "OH2", [128, NT, E], F32)
                SV = Buf(P, stack, "SV", [128, 8, E], F32)
                DEN = Buf(P, stack, "DEN", [128, NT, 1], F32)
                SLF = Buf(P, stack, "SLF", [128, 2, NT], F32)
                EB = Buf(P, stack, "EB", [128, 3, NSB], F32)
                I13F = Buf(P, stack, "I13F", [128, NSB, NG13], F32)
                I2F = Buf(P, stack, "I2F", [128, NSB, 2 * NJG], F32)
                ZI = Buf(P, stack, "ZI", [128, NC4], I32)
                SMT = Buf(P, stack, "SMT", [NC4, 128], I32)
                SMTF = Buf(P, stack, "SMTF", [NC4, 128], F32)
                P.dma("sp", RW.t[:], router_d[jm].rearrange("(k p) e -> p k e", p=128), hsem(), w=[RW.r0])
                P.op("dve", lambda h: h.tensor_copy(out=RWH.t[:], in_=RW.t[:]), r=[RW.r0], w=[RWH.r0])
                P.op("dve", lambda h: h.tensor_tensor(out=RWL.t[:], in0=RW.t[:], in1=RWH.t[:], op=ALU.subtract),
                     r=[RW.r0, RWH.r0], w=[RWL.r0])
                P.op("dve", lambda h: h.memset(ZI.t[:], 0), w=[ZI.r0])
                P.dma("sp", SLOTMAP.rearrange("(p c) o -> p (c o)", p=128), ZI.t[:], s_sm, r=[ZI.r0], w=[SMr])
                for b in range(NB):
                    to_fm(XT, b, lo=LO)
                    for tt in range(4):
                        t = 4 * b + tt
                        P.dma("sp", X1D[t * 128:(t + 1) * 128, :], X[:, t, :], s_x1d, r=[Xr[t]], w=[X1Dr[t]])
                        bk = nb()

                        def rt(h, tt=tt, bk=bk):
                            n = 0
                            for kc in range(KC):
                                hi = XT.t[:, kc, tt * 128:(tt + 1) * 128]
                                lo = LO.t[:, tt, kc, :]
                                for (a, wv) in ((hi, RWH), (lo, RWH), (hi, RWL)):
                                    ins = h.matmul(bk[0][:, 0:E], lhsT=a, rhs=wv.t[:, kc, :], start=(n == 0), stop=(n == 3 * KC - 1))
                                    n += 1
                            return ins
                        P.op("pe", rt, r=[RWH.r0, RWL.r0, XT.r[2 * tt], XT.r[2 * tt + 1], LO.r[2 * tt], LO.r[2 * tt + 1]], w=[bk[1]])
                        lg, m8, mk, gt, sm, oh1 = LG.t[:, t, :], M8.t[:, t, :], MK.t[:, t, :], GT.t[:, t, :], SM.t[:, t, :], OH1.t[:, t, :]
                        P.op("dve", lambda h, bk=bk, lg=lg: h.tensor_copy(out=lg, in_=bk[0][:, 0:E]), r=[bk[1]], w=[LG.r[t]])
                        P.op("dve", lambda h, lg=lg, m8=m8: h.max(out=m8, in_=lg), r=[LG.r[t]], w=[M8.r[t]])
                bc = lambda ap: ap.to_broadcast([128, NT, E])
                m1, m2 = M8.t[:, :, 0:1], M8.t[:, :, 1:2]
                P.op("dve", lambda h: h.tensor_tensor(out=MK.t[:], in0=LG.t[:], in1=bc(m2), op=ALU.is_ge), r=LG.r + M8.r, w=MK.r)
                P.op("dve", lambda h: h.tensor_tensor(out=OH1.t[:], in0=LG.t[:], in1=bc(m1), op=ALU.is_equal), r=LG.r + M8.r, w=OH1.r)
                P.op("dve", lambda h: h.tensor_tensor(out=GT.t[:], in0=LG.t[:], in1=bc(m1), op=ALU.subtract), r=LG.r + M8.r, w=GT.r)
                P.op("act", lambda h: h.activation(out=GT.t[:], in_=GT.t[:], func=AF.Exp), w=GT.r)
                P.op("dve", lambda h: h.tensor_tensor(out=GT.t[:], in0=GT.t[:], in1=MK.t[:], op=ALU.mult), r=MK.r, w=GT.r)
                P.op("dve", lambda h: h.reduce_sum(out=DEN.t[:].rearrange("p t o -> p (t o)"), in_=GT.t[:], axis=AX), r=GT.r, w=[DEN.r0])
                P.op("dve", lambda h: h.reciprocal(out=DEN.t[:], in_=DEN.t[:]), w=[DEN.r0])
                P.op("dve", lambda h: h.tensor_tensor(out=GT.t[:], in0=GT.t[:], in1=bc(DEN.t[:, :, 0:1]), op=ALU.mult), r=[DEN.r0], w=GT.r)
                fl = lambda buf: buf.t[:].rearrange("p t e -> p (t e)")
                bC, bW = nb(), nb()
                P.op("pe", lambda h: h.matmul(bC[0][:, 0:NT * E], lhsT=ones, rhs=fl(MK), start=True, stop=True), r=[CONST.r0] + MK.r, w=[bC[1]])
                P.op("pe", lambda h: h.matmul(bW[0][:, 0:NT * E], lhsT=tri, rhs=fl(MK), start=True, stop=True), r=[CONST.r0] + MK.r, w=[bW[1]])
                P.op("dve", lambda h: h.tensor_copy(out=fl(CS), in_=bC[0][:, 0:NT * E]), r=[bC[1]], w=[CS.r0])
                P.op("dve", lambda h: h.tensor_copy(out=fl(WS), in_=bW[0][:, 0:NT * E]), r=[bW[1]], w=[WS.r0])
                P.op("dve", lambda h: h.memset(OFF.t[:, 0, :], 0.0), w=[OFF.r0])
                for t in range(1, NT):
                    P.op("dve", lambda h, t=t: h.tensor_tensor(out=OFF.t[:, t, :], in0=OFF.t[:, t - 1, :], in1=CS.t[:, t - 1, :], op=ALU.add),
                         r=[CS.r0], w=[OFF.r0])
                cnt, nbk, end, b5 = SV.t[:, 0, :], SV.t[:, 1, :], SV.t[:, 2, :], SV.t[:, 3, :]
                P.op("dve", lambda h: h.tensor_tensor(out=cnt, in0=OFF.t[:, NT - 1, :], in1=CS.t[:, NT - 1, :], op=ALU.add),
                     r=[OFF.r0, CS.r0], w=[SV.r0])
                P.op("dve", lambda h: h.tensor_scalar(out=nbk, in0=cnt, scalar1=0.0, scalar2=None, op0=ALU.is_gt), w=[SV.r0])
                for j in range(1, NB):
                    P.op("dve", lambda h, j=j: h.scalar_tensor_tensor(out=nbk, in0=cnt, scalar=512.0 * j, in1=nbk, op0=ALU.is_gt, op1=ALU.add),
                         w=[SV.r0])
                P.op("dve", lambda h: h.tensor_copy(out=end[:, 0:1], in_=nbk[:, 0:1]), w=[SV.r0])
                for e in range(1, E):
                    P.op("dve", lambda h, e=e: h.tensor_tensor(out=end[:, e:e + 1], in0=end[:, e - 1:e], in1=nbk[:, e:e + 1], op=ALU.add),
                         w=[SV.r0])
                P.op("dve", lambda h: h.tensor_copy(out=NBTI.t[:], in_=end[:, E - 1:E]), r=[SV.r0], w=[NBTI.r0])
                P.op("dve", lambda h: h.tensor_tensor(out=b5, in0=end, in1=nbk, op=ALU.subtract), w=[SV.r0])
                P.op("dve", lambda h: h.tensor_scalar(out=b5, in0=b5, scalar1=512.0, scalar2=-1.0, op0=ALU.mult, op1=ALU.add), w=[SV.r0])
                P.op("dve", lambda h: h.tensor_tensor(out=fl(SLOT), in0=fl(OFF), in1=fl(WS), op=ALU.add), r=[OFF.r0, WS.r0], w=[SLOT.r0])
                for t in range(NT):
                    P.op("dve", lambda h, t=t: h.tensor_tensor(out=SLOT.t[:, t, :], in0=SLOT.t[:, t, :], in1=b5, op=ALU.add),
                         r=[SV.r0], w=[SLOT.r0])
                P.op("dve", lambda h: h.tensor_tensor(out=fl(OH2), in0=fl(MK), in1=fl(OH1), op=ALU.subtract), r=MK.r + OH1.r, w=[OH2.r0])
                for (oh, ohr, other, otherr, dst) in ((OH1, OH1.r, SLOT, [SLOT.r0], SLF.t[:, 0, :]), (OH2, [OH2.r0], SLOT, [SLOT.r0], SLF.t[:, 1, :]),
                                                      (OH1, OH1.r, GT, GT.r, G12.t[:, 0, :]), (OH2, [OH2.r0], GT, GT.r, G12.t[:, 1, :])):
                    dres = SLF.r0 if other is SLOT else G12.r0
                    P.op("dve", lambda h, oh=oh, other=other: h.tensor_tensor(out=fl(PR), in0=fl(oh), in1=fl(other), op=ALU.mult),
                         r=list(ohr) + list(otherr), w=[PR.r0])
                    P.op("dve", lambda h, dst=dst: h.reduce_sum(out=dst, in_=PR.t[:], axis=AX), r=[PR.r0], w=[dres])
                P.op("dve", lambda h: h.tensor_copy(out=SL1I.t[:], in_=SLF.t[:, 0, :]), r=[SLF.r0], w=[SL1I.r0])
                P.op("dve", lambda h: h.tensor_copy(out=SL2I.t[:], in_=SLF.t[:, 1, :]), r=[SLF.r0], w=[SL2I.r0])
                eb, eb13, eb2 = EB.t[:, 0, :], EB.t[:, 1, :], EB.t[:, 2, :]
                P.op("dve", lambda h: h.memset(eb, 0.0), w=[EB.r0])
                for e in range(E):
                    P.op("dve", lambda h, e=e: h.scalar_tensor_tensor(out=eb, in0=biota, scalar=end[:, e:e + 1], in1=eb, op0=ALU.is_ge, op1=ALU.add),
                         r=[SV.r0, CONST.r0], w=[EB.r0])
                P.op("dve", lambda h: h.tensor_scalar(out=eb, in0=eb, scalar1=float(E - 1), scalar2=None, op0=ALU.min), w=[EB.r0])
                P.op("dve", lambda h: h.tensor_scalar(out=eb13, in0=eb, scalar1=float(NG13 * 128), scalar2=float(jm * E * NG13 * 128),
                                                      op0=ALU.mult, op1=ALU.add), w=[EB.r0])
                P.op("dve", lambda h: h.tensor_scalar(out=eb2, in0=eb, scalar1=float(2 * NJG * 128), scalar2=float(jm * E * 2 * NJG * 128),
                                                      op0=ALU.mult, op1=ALU.add), w=[EB.r0])
                for b in range(NSB):
                    P.op("dve", lambda h, b=b: h.tensor_scalar(out=I13F.t[:, b, :], in0=c13, scalar1=eb13[:, b:b + 1], scalar2=None, op0=ALU.add),
                         r=[EB.r0, CONST.r0], w=[I13F.r0])
                    P.op("dve", lambda h, b=b: h.tensor_scalar(out=I2F.t[:, b, :], in0=c2, scalar1=eb2[:, b:b + 1], scalar2=None, op0=ALU.add),
                         r=[EB.r0, CONST.r0], w=[I2F.r0])
                P.op("dve", lambda h: h.tensor_copy(out=IDX13.t[:], in_=I13F.t[:]), r=[I13F.r0], w=[IDX13.r0])
                P.op("dve", lambda h: h.tensor_copy(out=IDX2.t[:], in_=I2F.t[:]), r=[I2F.r0], w=[IDX2.r0])
                s_sc = dsem()
                for t in range(NT):
                    for sl in (SL1I, SL2I):
                        P.idma(SLOTMAP[:, :], TOKID.t[:, t:t + 1], s_sc, out_off=sl.t[:, t:t + 1], r=[sl.r0, TOKID.r0, SMr], w=[])
                SMr.lw = (s_sc, P.semval[s_sc])
                P.dma("sp", SMT.t[:], SLOTMAP.rearrange("(c p) o -> c (p o)", p=128), hsem(), r=[SMr], w=[SMT.r0])
                P.op("dve", lambda h: h.tensor_copy(out=SMTF.t[:], in_=SMT.t[:]), r=[SMT.r0], w=[SMTF.r0])
                bT = nb()
                P.op("pe", lambda h: h.transpose(out=bT[0][:, 0:NC4], in_=SMTF.t[:], identity=CONST.t[0:NC4, 0:NC4]),
                     r=[SMTF.r0, CONST.r0], w=[bT[1]])
                P.op("dve", lambda h: h.tensor_copy(out=GIDX.t[:], in_=bT[0][:, 0:NC4]), r=[bT[1]], w=[GIDX.r0])
                P.barrier()
            with ExitStack() as stack:
                ph = {}
                rings = make_rings(ph, stack)
                G = Buf(P, stack, "Ge", [128, NJ, 512], BF16, nres=NJ)
                XG = Buf(P, stack, "XG", [128, 4, D], F32, nres=4)
                xg_sem = [dsem() for _ in range(4)]
                XTs = [Buf(P, stack, f"XTe{i}", [128, KC, 512], BF16, nres=8) for i in range(2)]
                YS = Buf(P, stack, "YS", [128, 4, D], F32, nres=4)
                nbt = nc.values_load(NBTI.t[0:1, 0:1])
                retired = frozenset([sl[1] for ring in rings for sl in ring] + list(xg_sem))
                NB_MIN = (2 * S) // 512

                def prep(b):
                    for tt in range(4):
                        P.idma(XG.t[:, tt, :], X1D[:, :], xg_sem[tt], in_off=GIDX.t[:, 4 * b + tt:4 * b + tt + 1], r=[GIDX.r0], w=[XG.r[tt]])
                    to_fm(XTs[b % 2], b, src=lambda tt: (XG.t[:, tt, :], XG.r[tt]))

                def do_block(b):
                    XT = XTs[b % 2]
                    if b == 0:
                        prep(0)
                    mid = (lambda b=b: prep(b + 1)) if b + 1 < NSB else None

                    def ld13(which, g, slot, b=b):
                        P.idma(slot[0].t[:].rearrange("p k f -> p (k f)"), (ew1_d, ew3_d)[which][:, :], slot[1],
                               in_off=IDX13.t[:, b, g:g + 1], r=[IDX13.r0], w=[slot[0].r0])

                    def ld2(hh, gi, j0, j1, slot, b=b):
                        P.idma(slot[0].t[:].rearrange("p j d -> p (j d)"), ew2_d[:, :], slot[1],
                               in_off=IDX2.t[:, b, hh * NJG + gi:hh * NJG + gi + 1], r=[IDX2.r0], w=[slot[0].r0])

                    def consume(tt, hh, bk, b=b):
                        evac(YS.t[:, tt, hh * 512:(hh + 1) * 512], bk[0][:], r=[bk[1]], w=[YS.r[tt]])
                        if hh == 1:
                            r0 = (4 * b + tt) * 128
                            P.dma("sp", YD[r0:r0 + 128, :], YS.t[:, tt, :], s_yd[tt], r=[YS.r[tt]], w=[])
                    ffn_block(ph, XT, ld13, ld2, FFE, rings, G, consume, mid=mid)
                for b in range(NSB):
                    if b < NB_MIN:
                        do_block(b)
                    else:
                        def fix(eng, si, delta, b=b):
                            if si in s_yd:
                                r0 = (4 * b + s_yd.index(si)) * 128
                                eng.h.dma_start(out=YD[r0:r0 + 128, :], in_=ZT.t[:]).then_inc(P.sems[si], delta)
                                return True
                            return False
                        P.cond(nbt > b, lambda b=b: do_block(b), JUNK.t[:], junk_d[:, :], fix=fix, retired=retired)
                P.retire(retired)
                for si in retired:
                    dsem_pool.remove(si)
                P.barrier()
            with ExitStack() as stack:
                Yb = [(Buf(P, stack, f"Y1_{i}", [128, D], F32), Buf(P, stack, f"Y2_{i}", [128, D], F32), dsem(), dsem())
                      for i in range(9)]
                lnt = [(Buf(P, stack, f"STe{i}", [128, 2, 6], F32), Buf(P, stack, f"MVe{i}", [128, 2], F32),
                        Buf(P, stack, f"SDe{i}", [128, 4], F32)) for i in range(9)]
                def combine(b):
                    gb = (LNB.t[:, 0, :], LNB.t[:, 1, :])

                    def A(t):
                        Y1, Y2, sy1, sy2 = Yb[t % 9]
                        P.idma(Y1.t[:], YD[:, :], sy1, in_off=SL1I.t[:, t:t + 1], r=[SL1I.r0], w=[Y1.r0])
                        P.idma(Y2.t[:], YD[:, :], sy2, in_off=SL2I.t[:, t:t + 1], r=[SL2I.r0], w=[Y2.r0])
                        P.op("act", lambda h: h.activation(out=Y1.t[:], in_=Y1.t[:], func=AF.Identity, scale=G12.t[:, 0, t:t + 1]),
                             r=[G12.r0], w=[Y1.r0])
                        P.op("dve", lambda h: h.scalar_tensor_tensor(out=Y1.t[:], in0=Y2.t[:], scalar=G12.t[:, 1, t:t + 1], in1=Y1.t[:],
                                                                     op0=ALU.mult, op1=ALU.add),
                             r=[G12.r0, Y2.r0], w=[Y1.r0])
                        P.op("dve", lambda h: h.scalar_tensor_tensor(out=Y1.t[:], in0=X[:, t, :], scalar=alpha, in1=Y1.t[:],
                                                                     op0=ALU.mult, op1=ALU.add),
                             r=[Xr[t]], w=[Y1.r0])
                        ln_stats(lnt[t % 9], Y1.t[:], [Y1.r0])

                    def B(t):
                        Y1 = Yb[t % 9][0]
                        ln_norm(lnt[t % 9], Y1.t[:], [Y1.r0])

                    def C(t):
                        Y1 = Yb[t % 9][0]
                        ln_affine(Y1.t[:], [Y1.r0], gb, LNB.r0, X[:, t, :], Xr[t])
                    t0 = 4 * b
                    A(t0)
                    A(t0 + 1)
                    B(t0)
                    A(t0 + 2)
                    B(t0 + 1)
                    C(t0)
                    A(t0 + 3)
                    B(t0 + 2)
                    C(t0 + 1)
                    B(t0 + 3)
                    C(t0 + 2)
                    C(t0 + 3)
                ple_phase(l, l == L - 1, pre=combine)

    def ple_parts(stack, l, nxt=2):
        PG = Buf(P, stack, "PG", [128, KC, D], BF16)
        PLW = Buf(P, stack, "PLW", [128, 2, D], BF16)
        PTs = [(Buf(P, stack, f"PT{i}", [128, 2, 512], BF16), dsem()) for i in range(nxt)]
        XTs = [Buf(P, stack, f"XTp{i}", [128, KC, 512], BF16, nres=8) for i in range(nxt)]
        SG = [Buf(P, stack, f"SG{i}", [128, 512], F32) for i in range(2)]
        sems = (dsem(), dsem())

        def start_loads():
            load_w("pool", PG, ple_gate_d[l].rearrange("(k p) f -> p k f", p=128), sems[0])
            load_w("pool", PLW, ple_w_d[l].rearrange("(k p) f -> p k f", p=128), sems[1])

        def emit1(b):
            XT = XTs[b % nxt]
            PT, ptsem = PTs[b % nxt]
            P.dma("pool", PT.t[:], pT_d[l][:, b * 512:(b + 1) * 512].rearrange("(k p) s -> p k s", p=128), ptsem, w=[PT.r0],
                  max_dma_last_dim=8192)
            to_fm(XT, b)

        def emit(b, last, after_fm=None):
            emit1(b)
            if after_fm is not None:
                after_fm()
            emit2(b, last)

        def emit2(b, last):
            XT = XTs[b % nxt]
            PT, ptsem = PTs[b % nxt]
            for tt in range(4):
                t = 4 * b + tt
                for hh in range(2):
                    bg, bp = nb(), nb()

                    def fg(h, tt=tt, hh=hh, bg=bg):
                        for kc in range(KC):
                            ins = h.matmul(bg[0][:], lhsT=XT.t[:, kc, tt * 128:(tt + 1) * 128], rhs=PG.t[:, kc, hh * 512:(hh + 1) * 512],
                                           start=(kc == 0), stop=(kc == KC - 1))
                        return ins
                    P.op("pe", fg, r=[PG.r0, XT.r[2 * tt], XT.r[2 * tt + 1]], w=[bg[1]])

                    def fp(h, tt=tt, hh=hh, bp=bp):
                        for kc in range(2):
                            ins = h.matmul(bp[0][:], lhsT=PT.t[:, kc, tt * 128:(tt + 1) * 128], rhs=PLW.t[:, kc, hh * 512:(hh + 1) * 512],
                                           start=(kc == 0), stop=(kc == 1))
                        return ins
                    P.op("pe", fp, r=[PLW.r0, PT.r0], w=[bp[1]])
                    sg = SG[hh]
                    P.op("act", lambda h, bg=bg, sg=sg: h.activation(out=sg.t[:], in_=bg[0][:], func=AF.Sigmoid), r=[bg[1]], w=[sg.r0])
                    P.op("dve", lambda h, bp=bp, sg=sg: h.tensor_tensor(out=sg.t[:], in0=bp[0][:], in1=sg.t[:], op=ALU.mult),
                         r=[bp[1]], w=[sg.r0])
                    xo = X[:, t, hh * 512:(hh + 1) * 512]
                    P.op("dve", lambda h, xo=xo, sg=sg: h.tensor_tensor(out=xo, in0=xo, in1=sg.t[:], op=ALU.add),
                         r=[sg.r0] + XT.r, w=[Xr[t]])
                if last:
                    P.dma("sp", out_d[t * 128:(t + 1) * 128, :], X[:, t, :], s_out, r=[Xr[t]])
        return emit, start_loads, emit1, emit2

    def ple_phase(l, last, pre=None):
        with ExitStack() as stack:
            emit, start_loads, _, _ = ple_parts(stack, l)
            if pre is not None:
                pre(0)
            start_loads()
            for b in range(NB):
                emit(b, last, after_fm=(lambda b=b: pre(b + 1)) if (pre is not None and b + 1 < NB) else None)
            P.barrier()

    for l in range(L):
        mix_phase(l)
        if l % 2 == 0:
            dense_phase(l)
        else:
            moe_phase(l)
    nc.sync.wait_ge(P.sems[s_out], P.semval[s_out])
    P.st.close()
    return nc


def prep_shared(cfg, inp):
    L, E, FFD, FFE = cfg.L, cfg.E, cfg.FFD, cfg.FFE
    f32 = np.float32
    sh = {}
    for k in ("w_in", "pool_w", "conv_pw", "w_out", "ple_gate_w", "ple_w", "dense_w2"):
        sh[k] = np.ascontiguousarray(inp[k], dtype=f32)

    def w13(w, FF):
        n = w.shape[0]
        return np.ascontiguousarray(w.reshape(n, KC, 128, FF // 256, 256).transpose(0, 3, 2, 1, 4).reshape(n, FF // 256, 128, KC * 256))
    sh["dense_w1"] = w13(np.asarray(inp["dense_w1"], f32), FFD)
    sh["dense_w3"] = w13(np.asarray(inp["dense_w3"], f32), FFD)
    if L // 2:
        NM = L // 2
        NE = NM * E
        sh["router_w"] = np.ascontiguousarray(inp["router_w"], dtype=f32)
        sh["exp_w1"] = w13(np.asarray(inp["exp_w1"], f32).reshape(NE, D, FFE), FFE).reshape(NE * (FFE // 256) * 128, KC * 256)
        sh["exp_w3"] = w13(np.asarray(inp["exp_w3"], f32).reshape(NE, D, FFE), FFE).reshape(NE * (FFE // 256) * 128, KC * 256)
        w2 = np.asarray(inp["exp_w2"], f32).reshape(NE, FFE // 512, 4, 128, 2, 512)
        sh["exp_w2"] = np.ascontiguousarray(w2.transpose(0, 4, 1, 3, 2, 5)).reshape(NE * 2 * (FFE // 512) * 128, 4 * 512)
    pp = np.zeros((128, L * NPP), f32)
    for l in range(L):
        o = l * NPP
        pp[:, o + 0:o + 4] = np.asarray(inp["pool_scale"][l], f32).reshape(4, 128).T
        pp[:, o + 4:o + 8] = np.asarray(inp["conv_b"][l], f32).reshape(4, 128).T
        pp[:, o + 8:o + 12] = np.asarray(inp["conv_ln_g"][l], f32).reshape(4, 128).T
        pp[:, o + 12:o + 16] = np.asarray(inp["conv_ln_b"][l], f32).reshape(4, 128).T
        cw = np.asarray(inp["conv_w"][l], f32)
        pp[:, o + 16:o + 16 + 4 * CW] = cw.reshape(CW, 4, 128).transpose(2, 1, 0).reshape(128, 4 * CW)
    sh["pp"] = pp
    lnb = np.zeros((L * 4, D), f32)
    for l in range(L):
        lnb[4 * l + 0] = inp["ln1_g"][l]
        lnb[4 * l + 1] = inp["ln1_b"][l]
        lnb[4 * l + 2] = inp["ln2_g"][l]
        lnb[4 * l + 3] = inp["ln2_b"][l]
    sh["lnb"] = lnb
    NSB = (2 * cfg.S + E * 511) // 512
    NG13, NJG = FFE // 256, FFE // 512
    CO = const_offsets(NSB, NG13, NJG)
    consts = np.zeros((128, CO["n"]), f32)
    consts[:, 0:128] = np.eye(128, dtype=f32)
    consts[:, 128:256] = 1.0
    for g, w in enumerate(WINS):
        cnt = np.minimum(np.arange(HALO_P) + 1.0, float(w))
        consts[:, 256 + g * HALO_P:256 + (g + 1) * HALO_P] = (1.0 / cnt).astype(f32)[None, :]
    pidx = np.arange(128)
    consts[:, CO["tri"]:CO["tri"] + 128] = (pidx[:, None] <= pidx[None, :]).astype(f32)
    consts[:, CO["biota"]:CO["biota"] + NSB] = np.arange(NSB, dtype=f32)[None, :]
    consts[:, CO["c13"]:CO["c13"] + NG13] = (np.arange(NG13)[None, :] * 128 + pidx[:, None]).astype(f32)
    consts[:, CO["c2"]:CO["c2"] + 2 * NJG] = (np.arange(2 * NJG)[None, :] * 128 + pidx[:, None]).astype(f32)
    sh["junk"] = np.zeros((128, 1), f32)
    sh["tokid"] = (np.arange(cfg.NT)[None, :] * 128 + pidx[:, None]).astype(np.int32)
    sh["consts"] = consts
    return sh


def kernel(**inputs):
    cfg = Cfg()
    n = 8
    nc = build(cfg)
    sh = prep_shared(cfg, inputs)
    x = np.asarray(inputs["x"], np.float32)
    p = np.asarray(inputs["p"], np.float32)
    in_maps = []
    for c in range(n):
        m = dict(sh)
        m["x"] = np.ascontiguousarray(x[c])
        m["pT"] = np.ascontiguousarray(p[:, c].transpose(0, 2, 1))
        in_maps.append(m)
    res = run_bass_kernel_spmd(nc, in_maps, core_ids=list(range(n)))
    return np.stack([r["out"] for r in res.results], axis=0).astype(np.float32)
```
